# Optimizing a Trainium2 kernel written in Bass

```python
import math
import jax, jax.numpy as jnp
from jax import lax
import numpy as np

D_MODEL = 1024
BATCH = 4
SEQ = 4096
DEPTH = 4

f32 = jnp.float32
N_EVEN = (DEPTH + 1) // 2
N_ODD = DEPTH // 2
D_FF = 4 * D_MODEL
MIX_WIDTH = D_MODEL
CONV_W = 4
NORM_EPS = 1e-6

LRU_WIDTH = MIX_WIDTH // 2
LRU_BLOCKS = 8
LRU_BW = LRU_WIDTH // LRU_BLOCKS
LRU_C = 8.0
LRU_COLS = 2 * LRU_WIDTH

RWKV_WIDTH = MIX_WIDTH // 2
RWKV_HEAD = 64
RWKV_HEADS = RWKV_WIDTH // RWKV_HEAD
RWKV_DECAY_RANK = 64
RWKV_A_RANK = 64
RWKV_GATE_RANK = 128
RWKV_COLS = 3 * RWKV_WIDTH + RWKV_DECAY_RANK + RWKV_A_RANK + RWKV_GATE_RANK
RWKV_LN_EPS = 64e-5
EVEN_IN_COLS = LRU_COLS + RWKV_COLS

GDN_HEADS = 4
GDN_DK = 128
GDN_DV = 128
GDN_WIDTH = GDN_HEADS * GDN_DV
GDN_QKV = GDN_HEADS * (2 * GDN_DK + GDN_DV)
GDN_COLS = GDN_QKV + GDN_WIDTH + 2 * GDN_HEADS
GDN_CHUNK = 64

MLSTM_HEADS = 4
MLSTM_DK = 64
MLSTM_DV = 128
MLSTM_WIDTH = MLSTM_HEADS * MLSTM_DV
MLSTM_QK = 2 * MLSTM_HEADS * MLSTM_DK
MLSTM_COLS = MLSTM_QK + 2 * MLSTM_WIDTH + 2 * MLSTM_HEADS
MLSTM_CHUNK = 64
ODD_IN_COLS = GDN_COLS + MLSTM_COLS

kernel_name = 'hybrid_rglru_rwkv7_gdn_mlstm_trunk'


def rms_norm(x, g, eps=NORM_EPS):
    xf = x.astype(f32)
    y = xf * lax.rsqrt(jnp.mean(jnp.square(xf), axis=-1, keepdims=True) + eps)
    return (y * g.astype(f32)).astype(x.dtype)


def l2_normalize(x, eps=1e-6):
    xf = x.astype(f32)
    return xf * lax.rsqrt(jnp.sum(jnp.square(xf), axis=-1, keepdims=True) + eps)


def causal_dwconv(x, w):
    K, T = w.shape[0], x.shape[1]
    xp = jnp.pad(x, ((0, 0), (K - 1, 0), (0, 0)))
    return sum(xp[:, k:k + T] * w[k] for k in range(K))


def token_shift(x):
    return jnp.pad(x, ((0, 0), (1, 0), (0, 0)))[:, :-1]


def to_chunks(x, c):
    B, T, H = x.shape[:3]
    x = x.reshape((B, T // c, c, H) + x.shape[3:])
    return jnp.moveaxis(x, 3, 1)


def from_chunks(x):
    B, H, N, C, d = x.shape
    return jnp.moveaxis(x, 1, 3).reshape(B, N * C, H, d)


def chunk_major(x):
    return jnp.moveaxis(x, 2, 0)


def _lin_combine(left, right):
    a_l, b_l = left
    a_r, b_r = right
    return a_l * a_r, a_r * b_l + b_r


def griffin_recurrent(p, conv_w, conv_b, wa, ba, wx, bx, lam):
    gate_in, rec_in = p[..., :LRU_WIDTH], p[..., LRU_WIDTH:]
    xc = (causal_dwconv(rec_in, conv_w) + conv_b).astype(f32)
    B, T, _ = xc.shape
    xb = xc.reshape(B, T, LRU_BLOCKS, LRU_BW)
    r_gate = jax.nn.sigmoid(jnp.einsum('btnd,nde->btne', xb, wa).reshape(B, T, LRU_WIDTH) + ba)
    i_gate = jax.nn.sigmoid(jnp.einsum('btnd,nde->btne', xb, wx).reshape(B, T, LRU_WIDTH) + bx)
    log_a = -LRU_C * r_gate * jax.nn.softplus(-lam.astype(f32))
    a = jnp.exp(log_a)
    mult = jnp.sqrt(-jnp.expm1(2.0 * log_a))
    mult = jnp.where(jnp.arange(T)[None, :, None] == 0, 1.0, mult)
    u = i_gate * xc * mult
    _, h = lax.associative_scan(_lin_combine, (a, u), axis=1)
    return (h * jax.nn.gelu(gate_in.astype(f32), approximate=True)).astype(p.dtype)


def rwkv7_mix(p, mu, w0, w2, a0, a2, g2, k_k, k_a, r_k, ln_w, ln_b):
    W, RD, RA = RWKV_WIDTH, RWKV_DECAY_RANK, RWKV_A_RANK
    pf = p.astype(f32)
    pf = pf + mu * (token_shift(pf) - pf)
    r, k, v = pf[..., :W], pf[..., W:2 * W], pf[..., 2 * W:3 * W]
    xw = pf[..., 3 * W:3 * W + RD]
    xa = pf[..., 3 * W + RD:3 * W + RD + RA]
    xg = pf[..., 3 * W + RD + RA:]
    w_log = -jax.nn.softplus(-(w0 + jnp.tanh(xw) @ w2)) - 0.5
    decay = jnp.exp(-jnp.exp(w_log))
    a = jax.nn.sigmoid(a0 + xa @ a2)
    g = jax.nn.sigmoid(xg) @ g2
    B, T, _ = pf.shape
    heads = lambda t: t.reshape(B, T, RWKV_HEADS, RWKV_HEAD)
    kk = l2_normalize(heads(k * k_k))
    k = k * (1.0 + (a - 1.0) * k_a)
    r_h, k_h, v_h, a_h, w_h = heads(r), heads(k), heads(v), heads(a), heads(decay)

    def step(S, inp):
        rt, wt, kt, vt, kkt, at = inp
        sk = jnp.einsum('bhvk,bhk->bhv', S, kkt)
        S = (S * wt[:, :, None, :] - sk[..., None] * (kkt * at)[:, :, None, :]
             + vt[..., None] * kt[:, :, None, :])
        return S, jnp.einsum('bhvk,bhk->bhv', S, rt)

    tm = lambda t: jnp.moveaxis(t, 1, 0)
    S0 = jnp.zeros((B, RWKV_HEADS, RWKV_HEAD, RWKV_HEAD), f32)
    _, y = lax.scan(step, S0, (tm(r_h), tm(w_h), tm(k_h), tm(v_h), tm(kk), tm(a_h)))
    y = jnp.moveaxis(y, 0, 1)
    mean = jnp.mean(y, axis=-1, keepdims=True)
    var = jnp.mean(jnp.square(y - mean), axis=-1, keepdims=True)
    y = ((y - mean) * lax.rsqrt(var + RWKV_LN_EPS)).reshape(B, T, W) * ln_w + ln_b
    bonus = jnp.sum(r_h * k_h * r_k, axis=-1, keepdims=True) * v_h
    y = (y + bonus.reshape(B, T, W)) * g
    return y.astype(p.dtype)


def gated_delta_chunked(q, k, v, g, beta):
    B, T, H, DK = q.shape
    DV = v.shape[-1]
    C = GDN_CHUNK
    q = q * DK ** -0.5
    qc, kc, vc = to_chunks(q, C), to_chunks(k, C), to_chunks(v, C)
    gc = jnp.cumsum(to_chunks(g, C), axis=-1)
    bc = to_chunks(beta, C)
    causal = jnp.tril(jnp.ones((C, C), bool))
    strict = jnp.tril(jnp.ones((C, C), bool), -1)
    decay = jnp.exp(jnp.where(causal, gc[..., :, None] - gc[..., None, :], -jnp.inf))
    kb = kc * bc[..., None]
    A = jnp.where(strict, jnp.einsum('bhnik,bhnjk->bhnij', kb, kc) * decay, 0.0)
    u = lax.linalg.triangular_solve(A, vc * bc[..., None], left_side=True, lower=True, unit_diagonal=True)
    w = lax.linalg.triangular_solve(A, kb * jnp.exp(gc)[..., None], left_side=True, lower=True, unit_diagonal=True)
    qk = jnp.einsum('bhnik,bhnjk->bhnij', qc, kc) * decay
    q_dec = qc * jnp.exp(gc)[..., None]
    k_dec = kc * jnp.exp(gc[..., -1:] - gc)[..., None]
    g_last = jnp.exp(gc[..., -1])

    def step(S, inp):
        q_d, k_d, u_c, w_c, qk_c, gl = inp
        v_new = u_c - jnp.einsum('bhck,bhkv->bhcv', w_c, S)
        o = jnp.einsum('bhck,bhkv->bhcv', q_d, S) + jnp.einsum('bhij,bhjv->bhiv', qk_c, v_new)
        S = S * gl[..., None, None] + jnp.einsum('bhck,bhcv->bhkv', k_d, v_new)
        return S, o

    S0 = jnp.zeros((B, H, DK, DV), f32)
    _, o = lax.scan(step, S0, (chunk_major(q_dec), chunk_major(k_dec), chunk_major(u),
                               chunk_major(w), chunk_major(qk), chunk_major(g_last)))
    return from_chunks(jnp.moveaxis(o, 0, 2))


def gdn_mix(p, conv_w, a_log, dt_bias, norm_g):
    B, T, _ = p.shape
    H = GDN_HEADS
    hk = H * GDN_DK
    qkv = jax.nn.silu(causal_dwconv(p[..., :GDN_QKV], conv_w))
    q = l2_normalize(qkv[..., :hk].reshape(B, T, H, GDN_DK))
    k = l2_normalize(qkv[..., hk:2 * hk].reshape(B, T, H, GDN_DK))
    v = qkv[..., 2 * hk:].reshape(B, T, H, GDN_DV).astype(f32)
    o0 = GDN_QKV
    z = p[..., o0:o0 + GDN_WIDTH].reshape(B, T, H, GDN_DV).astype(f32)
    b_raw = p[..., o0 + GDN_WIDTH:o0 + GDN_WIDTH + H].astype(f32)
    a_raw = p[..., o0 + GDN_WIDTH + H:].astype(f32)
    beta = jax.nn.sigmoid(b_raw)
    g = -jnp.exp(a_log.astype(f32)) * jax.nn.softplus(a_raw + dt_bias)
    o = gated_delta_chunked(q, k, v, g, beta)
    o = rms_norm(o, norm_g) * jax.nn.silu(z)
    return o.reshape(B, T, GDN_WIDTH).astype(p.dtype)


def mlstm_chunked(q, k, v, i_pre, f_pre):
    B, T, H, DK = q.shape
    DV = v.shape[-1]
    C = MLSTM_CHUNK
    q = q * DK ** -0.5
    qc, kc, vc = to_chunks(q, C), to_chunks(k, C), to_chunks(v, C)
    b = jnp.cumsum(to_chunks(jax.nn.log_sigmoid(f_pre), C), axis=-1)
    ic = to_chunks(i_pre, C)
    causal = jnp.tril(jnp.ones((C, C), bool))
    D = jnp.where(causal, b[..., :, None] - b[..., None, :] + ic[..., None, :], -jnp.inf)
    d_max = jnp.max(D, axis=-1)
    to_end = b[..., -1:] - b + ic
    qk = jnp.einsum('bhnik,bhnjk->bhnij', qc, kc)

    def step(carry, inp):
        Cs, ns, m = carry
        q_c, k_c, v_c, b_c, D_c, dmax_c, te_c, qk_c = inp
        inter = b_c + m[..., None]
        m_row = jnp.maximum(inter, dmax_c)
        w_inter = jnp.exp(inter - m_row)
        Wm = jnp.exp(D_c - m_row[..., None]) * qk_c
        num = (w_inter[..., None] * jnp.einsum('bhck,bhkv->bhcv', q_c, Cs)
               + jnp.einsum('bhij,bhjv->bhiv', Wm, v_c))
        den = w_inter * jnp.einsum('bhck,bhk->bhc', q_c, ns) + jnp.sum(Wm, axis=-1)
        h = num / jnp.maximum(jnp.abs(den), jnp.exp(-m_row))[..., None]
        m_new = jnp.maximum(b_c[..., -1] + m, jnp.max(te_c, axis=-1))
        decay_prev = jnp.exp(b_c[..., -1] + m - m_new)
        wk = k_c * jnp.exp(te_c - m_new[..., None])[..., None]
        Cs = decay_prev[..., None, None] * Cs + jnp.einsum('bhck,bhcv->bhkv', wk, v_c)
        ns = decay_prev[..., None] * ns + jnp.sum(wk, axis=-2)
        return (Cs, ns, m_new), h

    init = (jnp.zeros((B, H, DK, DV), f32), jnp.zeros((B, H, DK), f32), jnp.zeros((B, H), f32))
    _, h = lax.scan(step, init, (chunk_major(qc), chunk_major(kc), chunk_major(vc), chunk_major(b),
                                 chunk_major(D), chunk_major(d_max), chunk_major(to_end), chunk_major(qk)))
    return from_chunks(jnp.moveaxis(h, 0, 2))


def mlstm_mix(p, conv_w, ig_b, fg_b, norm_g):
    B, T, _ = p.shape
    H, W = MLSTM_HEADS, MLSTM_WIDTH
    hk = H * MLSTM_DK
    qk = jax.nn.silu(causal_dwconv(p[..., :MLSTM_QK], conv_w))
    q = qk[..., :hk].reshape(B, T, H, MLSTM_DK).astype(f32)
    k = qk[..., hk:].reshape(B, T, H, MLSTM_DK).astype(f32)
    o0 = MLSTM_QK
    v = p[..., o0:o0 + W].reshape(B, T, H, MLSTM_DV).astype(f32)
    og = p[..., o0 + W:o0 + 2 * W].astype(f32)
    i_pre = p[..., o0 + 2 * W:o0 + 2 * W + H].astype(f32) + ig_b
    f_pre = p[..., o0 + 2 * W + H:].astype(f32) + fg_b
    h = mlstm_chunked(q, k, v, i_pre, f_pre)
    h = rms_norm(h, norm_g).reshape(B, T, W) * jax.nn.sigmoid(og)
    return h.astype(p.dtype)


def setup_inputs(seed: int = 0) -> dict:
    key = jax.random.key(seed)
    ks = iter(jax.random.split(key, 48))

    def nrm(shape, scale):
        return jax.random.normal(next(ks), shape, f32) * scale

    def uni(shape, lo, hi):
        return jax.random.uniform(next(ks), shape, f32, lo, hi)

    E, O, D = N_EVEN, N_ODD, D_MODEL
    x = nrm((BATCH, SEQ, D), 1.0)
    norm_mix_g = 1.0 + nrm((DEPTH, D), 0.02)
    norm_mlp_g = 1.0 + nrm((DEPTH, D), 0.02)
    mlp_up = nrm((DEPTH, D, D_FF), D ** -0.5)
    mlp_down = nrm((DEPTH, D_FF, D), D_FF ** -0.5)
    final_g = 1.0 + nrm((D,), 0.02)
    ev_w_in = nrm((E, D, EVEN_IN_COLS), D ** -0.5)
    ev_w_out = nrm((E, MIX_WIDTH, D), MIX_WIDTH ** -0.5)
    lru_conv_w = nrm((E, CONV_W, LRU_WIDTH), CONV_W ** -0.5)
    lru_conv_b = nrm((E, LRU_WIDTH), 0.02)
    lru_wa = nrm((E, LRU_BLOCKS, LRU_BW, LRU_BW), LRU_BW ** -0.5)
    lru_ba = nrm((E, LRU_WIDTH), 0.02)
    lru_wx = nrm((E, LRU_BLOCKS, LRU_BW, LRU_BW), LRU_BW ** -0.5)
    lru_bx = nrm((E, LRU_WIDTH), 0.02)
    a_base = uni((E, LRU_WIDTH), 0.9, 0.999) ** (1.0 / LRU_C)
    lru_lambda = jnp.log(a_base) - jnp.log1p(-a_base)
    rwkv_mu = uni((E, RWKV_COLS), 0.0, 1.0)
    rwkv_w0 = uni((E, RWKV_WIDTH), -6.0, -1.0)
    rwkv_w2 = nrm((E, RWKV_DECAY_RANK, RWKV_WIDTH), 0.1 * RWKV_DECAY_RANK ** -0.5)
    rwkv_a0 = nrm((E, RWKV_WIDTH), 0.1)
    rwkv_a2 = nrm((E, RWKV_A_RANK, RWKV_WIDTH), RWKV_A_RANK ** -0.5)
    rwkv_g2 = nrm((E, RWKV_GATE_RANK, RWKV_WIDTH), RWKV_GATE_RANK ** -0.5)
    rwkv_kk = 0.85 + nrm((E, RWKV_WIDTH), 0.05)
    rwkv_ka = 1.0 + nrm((E, RWKV_WIDTH), 0.05)
    rwkv_rk = nrm((E, RWKV_HEADS, RWKV_HEAD), 0.1)
    rwkv_lnw = 1.0 + nrm((E, RWKV_WIDTH), 0.02)
    rwkv_lnb = nrm((E, RWKV_WIDTH), 0.02)
    od_w_in = nrm((O, D, ODD_IN_COLS), D ** -0.5)
    od_w_out = nrm((O, MIX_WIDTH, D), MIX_WIDTH ** -0.5)
    gdn_conv_w = nrm((O, CONV_W, GDN_QKV), CONV_W ** -0.5)
    gdn_a_log = jnp.log(uni((O, GDN_HEADS), 1.0, 16.0))
    dt = jnp.exp(uni((O, GDN_HEADS), math.log(1e-3), math.log(1e-1)))
    gdn_dt_bias = dt + jnp.log(-jnp.expm1(-dt))
    gdn_norm_g = 1.0 + nrm((O, GDN_DV), 0.02)
    mlstm_conv_w = nrm((O, CONV_W, MLSTM_QK), CONV_W ** -0.5)
    mlstm_ig_b = nrm((O, MLSTM_HEADS), 0.1)
    mlstm_fg_b = jnp.linspace(3.0, 6.0, MLSTM_HEADS, dtype=f32)[None, :] + nrm((O, MLSTM_HEADS), 0.1)
    mlstm_norm_g = 1.0 + nrm((O, MLSTM_DV), 0.02)
    return {
        'x': x, 'norm_mix_g': norm_mix_g, 'norm_mlp_g': norm_mlp_g,
        'mlp_up': mlp_up, 'mlp_down': mlp_down, 'final_g': final_g,
        'ev_w_in': ev_w_in, 'ev_w_out': ev_w_out,
        'lru_conv_w': lru_conv_w, 'lru_conv_b': lru_conv_b,
        'lru_wa': lru_wa, 'lru_ba': lru_ba, 'lru_wx': lru_wx, 'lru_bx': lru_bx,
        'lru_lambda': lru_lambda,
        'rwkv_mu': rwkv_mu, 'rwkv_w0': rwkv_w0, 'rwkv_w2': rwkv_w2,
        'rwkv_a0': rwkv_a0, 'rwkv_a2': rwkv_a2, 'rwkv_g2': rwkv_g2,
        'rwkv_kk': rwkv_kk, 'rwkv_ka': rwkv_ka, 'rwkv_rk': rwkv_rk,
        'rwkv_lnw': rwkv_lnw, 'rwkv_lnb': rwkv_lnb,
        'od_w_in': od_w_in, 'od_w_out': od_w_out,
        'gdn_conv_w': gdn_conv_w, 'gdn_a_log': gdn_a_log, 'gdn_dt_bias': gdn_dt_bias,
        'gdn_norm_g': gdn_norm_g,
        'mlstm_conv_w': mlstm_conv_w, 'mlstm_ig_b': mlstm_ig_b, 'mlstm_fg_b': mlstm_fg_b,
        'mlstm_norm_g': mlstm_norm_g,
    }


def reference(x, norm_mix_g, norm_mlp_g, mlp_up, mlp_down, final_g,
              ev_w_in, ev_w_out, lru_conv_w, lru_conv_b, lru_wa, lru_ba, lru_wx, lru_bx,
              lru_lambda, rwkv_mu, rwkv_w0, rwkv_w2, rwkv_a0, rwkv_a2, rwkv_g2,
              rwkv_kk, rwkv_ka, rwkv_rk, rwkv_lnw, rwkv_lnb,
              od_w_in, od_w_out, gdn_conv_w, gdn_a_log, gdn_dt_bias, gdn_norm_g,
              mlstm_conv_w, mlstm_ig_b, mlstm_fg_b, mlstm_norm_g):
    for l in range(DEPTH):
        h = rms_norm(x, norm_mix_g[l])
        if l % 2 == 0:
            e = l // 2
            p = h @ ev_w_in[e]
            ya = griffin_recurrent(p[..., :LRU_COLS], lru_conv_w[e], lru_conv_b[e],
                                   lru_wa[e], lru_ba[e], lru_wx[e], lru_bx[e], lru_lambda[e])
            yb = rwkv7_mix(p[..., LRU_COLS:], rwkv_mu[e], rwkv_w0[e], rwkv_w2[e], rwkv_a0[e],
                           rwkv_a2[e], rwkv_g2[e], rwkv_kk[e], rwkv_ka[e], rwkv_rk[e],
                           rwkv_lnw[e], rwkv_lnb[e])
            y = jnp.concatenate([ya, yb], axis=-1) @ ev_w_out[e]
        else:
            o = l // 2
            p = h @ od_w_in[o]
            yc = gdn_mix(p[..., :GDN_COLS], gdn_conv_w[o], gdn_a_log[o], gdn_dt_bias[o], gdn_norm_g[o])
            yd = mlstm_mix(p[..., GDN_COLS:], mlstm_conv_w[o], mlstm_ig_b[o], mlstm_fg_b[o], mlstm_norm_g[o])
            y = jnp.concatenate([yc, yd], axis=-1) @ od_w_out[o]
        x = x + y
        h = rms_norm(x, norm_mlp_g[l])
        x = x + jnp.square(jax.nn.relu(h @ mlp_up[l])) @ mlp_down[l]
    return rms_norm(x, final_g)
```

```python
import numpy as np
from contextlib import ExitStack
import concourse.bass as bass
import concourse.mybir as mybir
from concourse.bass_utils import run_bass_kernel_spmd

F32 = mybir.dt.float32
BF16 = mybir.dt.bfloat16
AF = mybir.ActivationFunctionType
ALU = mybir.AluOpType
AX = mybir.AxisListType

P = 128
NT = 512
NBLK = NT // P
D = 1024
KC = D // P
DFF = 4096
EPOCH = 30000
WSLOTS = 3


class _Rec:
    def __init__(self):
        self.call = None

    def __getattr__(self, name):
        def f(*a, **k):
            self.call = (name, a, k)
            return self
        return f


class Sched:
    ENGS = ("pe", "dve", "act", "pool", "sp")

    def __init__(self, nc):
        self.nc = nc
        self.semh = {}
        self.cnt = {e: 0 for e in self.ENGS}
        self.seen = {e: {} for e in self.ENGS}
        self.prog = {e: [] for e in self.ENGS}
        self.lastw = {}
        self.readers = {}
        self.dmaval = {}
        self.nwaits = 0

    def _sem(self, key):
        if key not in self.semh:
            self.semh[key] = self.nc.alloc_semaphore(name="s_" + str(key).replace(":", "_").replace(".", "_"))
        return self.semh[key]

    def _need(self, eng, tok):
        sk, v = tok
        if self.seen[eng].get(sk, 0) < v:
            self.seen[eng][sk] = v
            self.prog[eng].append(("wait", self._sem(sk), v))
            self.nwaits += 1

    def op(self, eng, fn, r=(), w=(), dma=None):
        rec = _Rec()
        fn(rec)
        assert rec.call is not None
        fn = rec.call
        deps = []
        for k in r:
            t = self.lastw.get(k)
            if t is not None:
                deps.append((t, "raw"))
        for k in w:
            t = self.lastw.get(k)
            if t is not None:
                deps.append((t, "waw"))
            for t in self.readers.get(k, {}).items():
                deps.append((t, "war"))
        for (tok, kind) in deps:
            sk = tok[0]
            own = isinstance(sk, tuple) and sk[0] == eng
            if own and (eng == "pe" or (eng in ("dve", "act") and kind != "raw")):
                continue
            self._need(eng, tok)
        if dma is None:
            self.cnt[eng] += 1
            ep = self.cnt[eng] // EPOCH
            sk = (eng, ep)
            val = self.cnt[eng] - ep * EPOCH
            if val == 0:
                sk = (eng, ep - 1)
                val = EPOCH
            tok = (sk, val)
            self.prog[eng].append(("op", fn, self._sem(sk), 1))
        else:
            sk = "dma:" + dma
            self.dmaval[sk] = self.dmaval.get(sk, 0) + 16
            tok = (sk, self.dmaval[sk])
            self.prog[eng].append(("op", fn, self._sem(sk), 16))
        for k in w:
            self.lastw[k] = tok
            self.readers[k] = {}
        for k in r:
            d = self.readers.setdefault(k, {})
            if d.get(tok[0], 0) < tok[1]:
                d[tok[0]] = tok[1]
        return tok

    def wait_all(self, eng, keys):
        for k in keys:
            t = self.lastw.get(k)
            if t is not None:
                self._need(eng, t)

    def finalize(self):
        nc = self.nc
        engs = dict(pe="tensor", dve="vector", act="scalar", pool="gpsimd", sp="sync")

        def replay(name, e):
            for item in self.prog[name]:
                if item[0] == "wait":
                    e.wait_ge(item[1], item[2])
                else:
                    name_, a_, k_ = item[1]
                    ins = getattr(e, name_)(*a_, **k_)
                    ins.then_inc(item[2], item[3])

        with nc.Block() as block:
            for name in self.ENGS:
                getattr(block, engs[name])(lambda e, name=name: replay(name, e))


LRU_C = 8.0
EV_COLS = 2816
OD_COLS = 3600


class MK:
    def __init__(self, T, layers, parts=("a", "b")):
        self.T = T
        self.layers = layers
        self.parts = parts
        self.nc = bass.Bass("TRN2", target_bir_lowering=False)
        self.S = Sched(self.nc)
        self.es = ExitStack()
        self.psn = 0
        self.pbn = 0
        self.tmpn = 0
        self.in_names = []

    def sb(self, name, shape, dt=F32):
        return self.es.enter_context(self.nc.sbuf_tensor(name, list(shape), dt))

    def dram_in(self, name, shape, dt=F32):
        self.in_names.append(name)
        return self.nc.dram_tensor(name, list(shape), dt, kind="ExternalInput")

    def dram_tmp(self, name, shape, dt=BF16):
        return self.nc.dram_tensor(name, list(shape), dt, kind="Internal")

    def psum(self):
        i = self.psn % 5
        self.psn += 1
        return self.ps[i], "ps%d" % i

    def psum_long(self):
        return self.ps[5], "ps5"

    def psumb(self):
        i = self.pbn % 2
        self.pbn += 1
        return self.psb[i], "psb%d" % i

    def E(self, eng, fn, r=(), w=()):
        return self.S.op(eng, fn, r=r, w=w)

    def tap(self, name, ap, key, shape, dt=F32):
        if not getattr(self, "taps_on", False):
            return
        if not hasattr(self, "tapd"):
            self.tapd = {}
        if name in self.tapd:
            return
        t = self.nc.dram_tensor("dbg_" + name, list(shape), dt, kind="ExternalOutput")
        self.tapd[name] = t
        self.S.op("sp", lambda e: e.dma_start(out=t.ap(), in_=ap), r=[key], w=["dbg_" + name], dma="dbg_" + name)
        self.S.wait_all("sp", ["dbg_" + name])

    def fs(self, i):
        if i < 8:
            return self.xtok[:, i // 2, (i % 2) * 512:(i % 2 + 1) * 512], "fs.%d" % i
        return self.fsx[:, i - 8, :], "fs.%d" % i

    def bs(self, i):
        return self.uT[:, i, :], "uT.%d" % i

    def build(self):
        nc, S = self.nc, self.S
        T = self.T
        NTILES = T // NT
        self.x = self.dram_in("x", [T, D])
        self.out = nc.dram_tensor("out", [T, D], F32, kind="ExternalOutput")
        self.d_final_g = self.dram_in("final_g", [D])
        self.d_cmask = self.dram_in("cmask", [P, 11 * P], BF16)

        self.xT = self.sb("xT", [P, KC, NT], F32)
        self.hT = self.sb("hT", [P, KC, NT], BF16)
        self.uT = self.sb("uT", [P, DFF // P, NT], BF16)
        self.ymT = self.sb("ymT", [P, KC, NT], BF16)
        self.rstd = self.sb("rstd", [P, NT], F32)
        self.tmpb = [self.sb("tmpb%d" % i, [P, NT], BF16) for i in range(3)]
        self.xtok = self.sb("xtok", [P, NBLK, D], F32)
        self.fsx = self.sb("fsx", [P, 12, 512], F32)
        self.tmpc = self.sb("tmpc", [P, 520], F32)
        self.wbuf = [self.sb("wbuf%d" % i, [P, KC, 512], BF16) for i in range(WSLOTS)]
        self.ident = self.sb("ident", [P, P], F32)
        self.ident_bf = self.sb("ident_bf", [P, 4, P], BF16)
        self.ones_bf = self.sb("ones_bf", [P, P], BF16)
        self.ones_blk = self.sb("ones_blk", [P, P], BF16)
        self.ones32 = self.sb("ones32", [P, P], F32)
        self.triU = self.sb("triU", [P, P], F32)
        self.maskS = self.sb("maskS", [P, 4, P], BF16)
        self.maskI = self.sb("maskI", [P, 4, P], BF16)
        self.mask4 = self.sb("mask4", [P, 4, P], BF16)
        self.epsc = self.sb("epsc", [P, 8], F32)
        self.cols = self.sb("cols", [P, 640], F32)
        self.stage = self.sb("stage", [P, P], F32)
        self.ps = [self.es.enter_context(nc.psum_tensor("psf%d" % i, [P, 512], F32)) for i in range(6)]
        self.psb = [self.es.enter_context(nc.psum_tensor("psb%d" % i, [P, 1024], BF16)) for i in range(2)]
        self.FSALL = ["fs.%d" % i for i in range(8)]
        self.cmask = self.sb("cmask_sb", [P, 11 * P], BF16)
        S.op("sp", lambda e: e.dma_start(out=self.cmask[:, :], in_=self.d_cmask.ap()), w=["cmask"], dma="cmask")

        E = self.E
        E("pool", lambda e: e.memset(self.ident[:, :], 0.0), w=["ident"])
        E("pool", lambda e: e.affine_select(self.ident[:, :], self.ident[:, :], [[1, P]],
                                            ALU.not_equal, 1.0, base=0, channel_multiplier=-1),
          r=["ident"], w=["ident"])
        for g in range(4):
            E("pool", lambda e, g=g: e.tensor_copy(self.ident_bf[:, g, :], self.ident[:, :]), r=["ident"], w=["ident_bf"])
        E("pool", lambda e: e.memset(self.ones_bf[:, :], 1.0), w=["ones_bf"])
        E("pool", lambda e: e.memset(self.ones32[:, :], 1.0), w=["ones32"])
        E("pool", lambda e: e.memset(self.ones_blk[:, :], 0.0), w=["ones_blk"])
        E("pool", lambda e: e.memset(self.ones_blk[0:64, 0:64], 1.0), w=["ones_blk"])
        E("pool", lambda e: e.memset(self.ones_blk[64:128, 64:128], 1.0), w=["ones_blk"])
        E("pool", lambda e: e.memset(self.epsc[:, 0:1], 1e-6), w=["epsc"])
        E("pool", lambda e: e.memset(self.epsc[:, 1:2], 1.0), w=["epsc"])
        E("pool", lambda e: e.memset(self.epsc[:, 2:3], -0.5), w=["epsc"])
        E("pool", lambda e: e.memset(self.epsc[:, 3:4], 64e-5), w=["epsc"])
        E("pool", lambda e: e.memset(self.epsc[:, 4:8], 0.0), w=["epsc"])
        for g in range(4):
            E("pool", lambda e, g=g: e.affine_select(self.maskS[:, g, :], self.ones32[:, 0:P], [[1, P]],
                                                     ALU.is_gt, 0.0, base=0, channel_multiplier=-1),
              r=["ones32"], w=["maskS"])
            E("pool", lambda e, g=g: e.affine_select(self.maskI[:, g, :], self.ones32[:, 0:P], [[1, P]],
                                                     ALU.is_ge, 0.0, base=0, channel_multiplier=-1),
              r=["ones32"], w=["maskI"])
        E("pool", lambda e: e.affine_select(self.triU[:, :], self.ones32[:, 0:P], [[1, P]],
                                            ALU.is_ge, 0.0, base=0, channel_multiplier=-1), r=["ones32"], w=["triU"])
        E("pool", lambda e: e.tensor_scalar(self.mask4[:, 0, :], self.maskS[:, 0, :], -1.0, None, ALU.mult),
          r=["maskS"], w=["mask4"])
        E("pool", lambda e: e.tensor_copy(self.mask4[:, 1, :], self.maskS[:, 0, :]), r=["maskS"], w=["mask4"])
        E("pool", lambda e: e.tensor_copy(self.mask4[:, 2, :], self.maskI[:, 0, :]), r=["maskI"], w=["mask4"])
        E("pool", lambda e: e.tensor_copy(self.mask4[:, 3, :], self.maskI[:, 0, :]), r=["maskI"], w=["mask4"])

        self.colmap = {}
        self.ncol = 0
        self.load_cols("final_g", self.d_final_g.ap().rearrange("(k p) -> k p", p=P), 8)

        self.L = {}
        for (kind, l) in self.layers:
            self.setup_layer(kind, l)

        self.wplan = []
        for ti in range(NTILES):
            for (kind, l) in self.layers:
                self.plan_layer(kind, l)
        self.wn = 0
        self.wissued = 0

        for ti in range(NTILES):
            self.ti = ti
            self.load_tile(ti)
            for (kind, l) in self.layers:
                if kind == "even":
                    self.even_layer(l)
                elif kind == "odd":
                    self.odd_layer(l)
                self.mlp(l)
            self.store_tile(ti)
        S.wait_all("sp", ["out_dram"])
        S.finalize()
        return nc

    def load_cols(self, name, src_rows, nrows):
        S = self.S
        assert nrows <= P
        S.op("sp", lambda e: e.dma_start(out=self.stage[:nrows, :], in_=src_rows), w=["stage"], dma="stage")
        pst, pk = self.psum()
        S.op("pe", lambda e: e.transpose(pst[:, :nrows], self.stage[:nrows, :], self.ident[:nrows, :nrows]),
             r=["stage", "ident"], w=[pk])
        off = self.ncol
        S.op("dve", lambda e: e.tensor_copy(self.cols[:, off:off + nrows], pst[:, :nrows]), r=[pk], w=["cols"])
        self.colmap[name] = off
        self.ncol += nrows
        assert self.ncol <= 640

    def col(self, name, i=0):
        o = self.colmap[name] + i
        return self.cols[:, o:o + 1]

    def cast_dram(self, dst2, src2, key):
        self.S.op("pool", lambda e: e.dma_start(out=dst2, in_=src2), w=["wd:" + key], dma="cv_" + key)

    def cast_flat(self, dst, src, key):
        s2 = src.rearrange("a b -> (a b)").rearrange("(r c) -> r c", c=1024)
        d2 = dst.rearrange("a b -> (a b)").rearrange("(r c) -> r c", c=1024)
        self.cast_dram(d2, s2, key)

    def setup_layer(self, kind, l):
        nc = self.nc
        L = {}
        self.L[l] = L
        L["up"] = self.dram_in("mlp_up%d" % l, [D, DFF])
        L["down"] = self.dram_in("mlp_down%d" % l, [DFF, D])
        L["up_bf"] = self.dram_tmp("up_bf%d" % l, [D, DFF])
        L["down_bf"] = self.dram_tmp("down_bf%d" % l, [DFF, D])
        self.cast_flat(L["up_bf"].ap(), L["up"].ap(), "up%d" % l)
        self.cast_flat(L["down_bf"].ap(), L["down"].ap(), "down%d" % l)
        L["ng1"] = self.dram_in("norm_mix_g%d" % l, [D])
        L["ng2"] = self.dram_in("norm_mlp_g%d" % l, [D])
        self.load_cols("ng1_%d" % l, L["ng1"].ap().rearrange("(k p) -> k p", p=P), 8)
        self.load_cols("ng2_%d" % l, L["ng2"].ap().rearrange("(k p) -> k p", p=P), 8)
        if kind == "even":
            self.setup_even(l, L)
        elif kind == "odd":
            self.setup_odd(l, L)

    def plan_w(self, dram_ap_2d, r0, c0, ncols, key):
        src = dram_ap_2d[r0:r0 + 1024, c0:c0 + ncols].rearrange("(k p) c -> p k c", p=P)
        self.wplan.append((src, ncols, key))

    def plan_layer(self, kind, l):
        L = self.L[l]
        if kind == "even":
            w = L["win_bf"].ap()
            for c0, n in [(512, 512), (0, 512), (2560, 256), (1024, 512), (1536, 512), (2048, 512)]:
                self.plan_w(w, 0, c0, n, "win%d" % l)
            for h in range(2):
                self.plan_w(L["wout_bf"].ap(), 0, h * 512, 512, "wout%d" % l)
        elif kind == "odd":
            self.plan_odd(l, L)
        up = L["up_bf"].ap()
        dn = L["down_bf"].ap()
        for b in range(8):
            self.plan_w(up, 0, b * 512, 512, "up%d" % l)
        for half in range(2):
            for g in range(4):
                self.plan_w(dn, g * 1024, half * 512, 512, "down%d" % l)

    def get_w(self):
        S = self.S
        n = self.wn
        self.wn += 1
        while self.wissued < min(n + WSLOTS, len(self.wplan)):
            i = self.wissued
            src, ncols, key = self.wplan[i]
            slot = i % WSLOTS
            S.op("sp", lambda e, src=src, ncols=ncols, slot=slot: e.dma_start(out=self.wbuf[slot][:, :, :ncols], in_=src),
                 r=["wd:" + k_ for k_ in (key if isinstance(key, tuple) else (key,))], w=["wbuf%d" % slot], dma="wbuf%d" % slot)
            self.wissued += 1
        slot = n % WSLOTS
        return self.wbuf[slot], "wbuf%d" % slot

    def load_tile(self, ti):
        S = self.S
        src = self.x.ap()[ti * NT:(ti + 1) * NT, :].rearrange("(b p) d -> p b d", p=P)
        S.op("sp", lambda e: e.dma_start(out=self.xtok[:, :, :], in_=src), w=self.FSALL, dma="xtok")
        for kc in range(KC):
            pst, pk = self.psum()
            for b in range(NBLK):
                S.op("pe", lambda e, b=b, kc=kc, pst=pst: e.transpose(
                    pst[:, b * P:(b + 1) * P], self.xtok[:, b, kc * P:(kc + 1) * P], self.ident[:, :]),
                    r=self.FSALL + ["ident"], w=[pk])
            S.op("dve", lambda e, kc=kc, pst=pst: e.tensor_copy(self.xT[:, kc, :], pst[:, :]),
                 r=[pk], w=["xT.%d" % kc])

    def rms_rstd(self):
        S = self.S
        for kc in range(KC):
            S.op("act", lambda e, kc=kc: e.activation(self.uT[:, 24 + kc, :], self.xT[:, kc, :], AF.Square),
                 r=["xT.%d" % kc], w=["uT.%d" % (24 + kc)])
        pst, pk = self.psum()
        for kc in range(KC):
            S.op("pe", lambda e, kc=kc: e.matmul(pst[:, :], self.ones_bf[:, :], self.uT[:, 24 + kc, :],
                                                 start=(kc == 0), stop=(kc == KC - 1)),
                 r=["uT.%d" % (24 + kc), "ones_bf"], w=[pk])
        S.op("act", lambda e: e.activation(self.rstd[:, :], pst[:, :], AF.Sqrt, bias=self.epsc[:, 0:1], scale=1.0 / D),
             r=[pk, "epsc"], w=["rstd"])
        S.op("dve", lambda e: e.reciprocal(self.rstd[:, :], self.rstd[:, :]), r=["rstd"], w=["rstd"])

    def rmsnorm_to_h(self, gname):
        S = self.S
        self.rms_rstd()
        for kc in range(KC):
            S.op("dve", lambda e, kc=kc: e.scalar_tensor_tensor(
                self.hT[:, kc, :], self.xT[:, kc, :], self.col(gname, kc), self.rstd[:, :],
                ALU.mult, ALU.mult),
                r=["xT.%d" % kc, "cols", "rstd"], w=["hT.%d" % kc])

    def proj_fm(self, w, wk, c0, ncols=P):
        pst, pk = self.psum()
        for kc in range(KC):
            self.E("pe", lambda e, kc=kc: e.matmul(pst[:ncols, :], w[:, kc, c0:c0 + ncols], self.hT[:, kc, :],
                                                   start=(kc == 0), stop=(kc == KC - 1)),
                   r=[wk, "hT.%d" % kc], w=[pk])
        return pst, pk

    def proj_tm(self, w, wk, c0, ncols, b):
        pst, pk = self.psum()
        for kc in range(KC):
            self.E("pe", lambda e, kc=kc: e.matmul(pst[:, :ncols], self.hT[:, kc, b * P:(b + 1) * P], w[:, kc, c0:c0 + ncols],
                                                   start=(kc == 0), stop=(kc == KC - 1)),
                   r=[wk, "hT.%d" % kc], w=[pk])
        return pst, pk

    def out_proj_residual(self):
        for half in range(2):
            w, wk = self.get_w()
            for c in range(4):
                pst, pk = self.psum()
                for kc in range(KC):
                    self.E("pe", lambda e, kc=kc, c=c, w=w, pst=pst: e.matmul(
                        pst[:, :], w[:, kc, c * P:(c + 1) * P], self.ymT[:, kc, :],
                        start=(kc == 0), stop=(kc == KC - 1)),
                        r=[wk, "ymT.%d" % kc], w=[pk])
                oc = half * 4 + c
                self.E("dve", lambda e, oc=oc, pst=pst: e.tensor_tensor(
                    self.xT[:, oc, :], pst[:, :], self.xT[:, oc, :], ALU.add),
                    r=[pk, "xT.%d" % oc], w=["xT.%d" % oc])

    def mlp(self, l):
        S = self.S
        self.rmsnorm_to_h("ng2_%d" % l)
        for blk in range(8):
            w, wk = self.get_w()
            for c in range(4):
                pst, pk = self.psum()
                for kc in range(KC):
                    S.op("pe", lambda e, kc=kc, c=c, w=w, pst=pst: e.matmul(
                        pst[:, :], w[:, kc, c * P:(c + 1) * P], self.hT[:, kc, :],
                        start=(kc == 0), stop=(kc == KC - 1)),
                        r=[wk, "hT.%d" % kc], w=[pk])
                tmp = self.tmpb[self.tmpn % 3]
                tk = "tmpb%d" % (self.tmpn % 3)
                self.tmpn += 1
                S.op("act", lambda e, tmp=tmp, pst=pst: e.activation(tmp[:, :], pst[:, :], AF.Relu), r=[pk], w=[tk])
                fc = blk * 4 + c
                S.op("pool", lambda e, tmp=tmp, fc=fc: e.tensor_tensor(self.uT[:, fc, :], tmp[:, :], tmp[:, :], ALU.mult),
                     r=[tk], w=["uT.%d" % fc])
        for half in range(2):
            pss = [self.psum() for _ in range(4)]
            for g in range(4):
                w, wk = self.get_w()
                for c in range(4):
                    for kc in range(KC):
                        fc = g * 8 + kc
                        S.op("pe", lambda e, kc=kc, c=c, w=w, fc=fc, pst=pss[c][0], g=g: e.matmul(
                            pst[:, :], w[:, kc, c * P:(c + 1) * P], self.uT[:, fc, :],
                            start=(g == 0 and kc == 0), stop=(g == 3 and kc == KC - 1)),
                            r=[wk, "uT.%d" % fc], w=[pss[c][1]])
            for c in range(4):
                oc = half * 4 + c
                S.op("dve", lambda e, oc=oc, pst=pss[c][0]: e.tensor_tensor(
                    self.xT[:, oc, :], pst[:, :], self.xT[:, oc, :], ALU.add),
                    r=[pss[c][1], "xT.%d" % oc], w=["xT.%d" % oc])

    def store_tile(self, ti):
        S = self.S
        self.rms_rstd()
        for kc in range(KC):
            S.op("dve", lambda e, kc=kc: e.scalar_tensor_tensor(
                self.xT[:, kc, :], self.xT[:, kc, :], self.col("final_g", kc), self.rstd[:, :],
                ALU.mult, ALU.mult),
                r=["xT.%d" % kc, "cols", "rstd"], w=["xT.%d" % kc])
        for b in range(NBLK):
            for q in range(2):
                pst, pk = self.psum()
                for j in range(4):
                    kc = q * 4 + j
                    S.op("pe", lambda e, b=b, kc=kc, j=j, pst=pst: e.transpose(
                        pst[:, j * P:(j + 1) * P], self.xT[:, kc, b * P:(b + 1) * P], self.ident[:, :]),
                        r=["xT.%d" % kc, "ident"], w=[pk])
                S.op("act", lambda e, b=b, q=q, pst=pst: e.activation(
                    self.xtok[:, b, q * 512:(q + 1) * 512], pst[:, :], AF.Copy),
                    r=[pk], w=["fs.%d" % (b * 2 + q)])
        dst = self.out.ap()[ti * NT:(ti + 1) * NT, :].rearrange("(b p) d -> p b d", p=P)
        S.op("sp", lambda e: e.dma_start(out=dst, in_=self.xtok[:, :, :]), r=self.FSALL, w=["out_dram"], dma="out")

    def tri_inverse(self, N0, N0k, G, scr):
        E = self.E
        GW = G * P
        (NT, NTk), (W, Wk), (WT, WTk), (NO, NOk), (NOT, NOTk), (XA, XAk), (XB, XBk) = scr[:7]
        cm = self.cmask
        v3 = lambda ap: ap[:, :GW].rearrange("p (g c) -> p g c", c=P)
        mk = lambda idx: cm[:, idx * P:(idx + 1) * P].unsqueeze(1).to_broadcast([P, G, P])
        idb = self.ident_bf[:, 0:G, :]
        pb, pbk = self.psumb()
        for g in range(G):
            E("pe", lambda e, g=g: e.transpose(pb[:, g * P:(g + 1) * P], N0[:, g * P:(g + 1) * P], self.ident_bf[:, 0, :]),
              r=[N0k, "ident_bf"], w=[pbk])
        E("act", lambda e: e.activation(NT[:, :GW], pb[:, :GW], AF.Copy), r=[pbk], w=[NTk])
        E("pool", lambda e: e.tensor_tensor(v3(NO), v3(N0), mk(0), ALU.mult), r=[N0k, "cmask"], w=[NOk])
        E("pool", lambda e: e.tensor_tensor(v3(NOT), v3(NT), mk(0), ALU.mult), r=[NTk, "cmask"], w=[NOTk])
        E("dve", lambda e: e.tensor_tensor(v3(W), v3(NO), idb, ALU.add), r=[NOk, "ident_bf"], w=[Wk])
        E("dve", lambda e: e.tensor_tensor(v3(WT), v3(NOT), idb, ALU.add), r=[NOTk, "ident_bf"], w=[WTk])
        p2, p2k = self.psum()
        for g in range(G):
            sl = slice(g * P, (g + 1) * P)
            E("pe", lambda e, sl=sl: e.matmul(p2[:, sl], NO[:, sl], NOT[:, sl], start=True, stop=True), r=[NOk, NOTk], w=[p2k])
        E("act", lambda e: e.activation(XA[:, :GW], p2[:, :GW], AF.Copy), r=[p2k], w=[XAk])
        p3, p3k = self.psum()
        p4, p4k = self.psum()
        for g in range(G):
            sl = slice(g * P, (g + 1) * P)
            E("pe", lambda e, sl=sl: e.matmul(p3[:, sl], XA[:, sl], W[:, sl], start=True, stop=True), r=[XAk, Wk], w=[p3k])
        for g in range(G):
            sl = slice(g * P, (g + 1) * P)
            E("pe", lambda e, sl=sl: e.matmul(p4[:, sl], W[:, sl], XA[:, sl], start=True, stop=True), r=[XAk, Wk], w=[p4k])
        E("dve", lambda e: e.tensor_tensor(W[:, :GW], p3[:, :GW], W[:, :GW], ALU.add), r=[p3k, Wk], w=[Wk])
        E("dve", lambda e: e.tensor_tensor(WT[:, :GW], p4[:, :GW], WT[:, :GW], ALU.add), r=[p4k, WTk], w=[WTk])
        for lv in range(5):
            last = (lv == 4)
            E("pool", lambda e: e.tensor_tensor(v3(NOT), v3(NT), mk(6 + lv), ALU.mult), r=[NTk, "cmask"], w=[NOTk])
            if not last:
                E("pool", lambda e: e.tensor_tensor(v3(NO), v3(N0), mk(1 + lv), ALU.mult), r=[N0k, "cmask"], w=[NOk])
            px, pxk = self.psum()
            for g in range(G):
                sl = slice(g * P, (g + 1) * P)
                E("pe", lambda e, sl=sl: e.matmul(px[:, sl], NOT[:, sl], W[:, sl], start=True, stop=True), r=[NOTk, Wk], w=[pxk])
            E("act", lambda e: e.activation(XA[:, :GW], px[:, :GW], AF.Copy), r=[pxk], w=[XAk])
            if not last:
                px2, px2k = self.psum()
                for g in range(G):
                    sl = slice(g * P, (g + 1) * P)
                    E("pe", lambda e, sl=sl: e.matmul(px2[:, sl], NO[:, sl], WT[:, sl], start=True, stop=True), r=[NOk, WTk], w=[px2k])
                E("act", lambda e: e.activation(XB[:, :GW], px2[:, :GW], AF.Copy), r=[px2k], w=[XBk])
            py_, pyk_ = self.psum()
            for g in range(G):
                sl = slice(g * P, (g + 1) * P)
                E("pe", lambda e, sl=sl: e.matmul(py_[:, sl], WT[:, sl], XA[:, sl], start=True, stop=True), r=[WTk, XAk], w=[pyk_])
            if not last:
                py2, py2k = self.psum()
                for g in range(G):
                    sl = slice(g * P, (g + 1) * P)
                    E("pe", lambda e, sl=sl: e.matmul(py2[:, sl], W[:, sl], XB[:, sl], start=True, stop=True), r=[Wk, XBk], w=[py2k])
            E("dve", lambda e: e.tensor_tensor(W[:, :GW], py_[:, :GW], W[:, :GW], ALU.add), r=[pyk_, Wk], w=[Wk])
            if not last:
                E("dve", lambda e: e.tensor_tensor(WT[:, :GW], py2[:, :GW], WT[:, :GW], ALU.add), r=[py2k, WTk], w=[WTk])
        return W, Wk

    def load_cols_multi(self, items):
        S = self.S
        off = 0
        for (name, ap, n) in items:
            S.op("sp", lambda e, ap=ap, off=off, n=n: e.dma_start(out=self.stage[off:off + n, :], in_=ap),
                 w=["stage"], dma="stage")
            self.colmap[name] = self.ncol + off
            off += n
        assert off <= P
        pst, pk = self.psum()
        S.op("pe", lambda e: e.transpose(pst[:, :off], self.stage[:off, :], self.ident[:off, :off]),
             r=["stage", "ident"], w=[pk])
        c0 = self.ncol
        S.op("dve", lambda e: e.tensor_copy(self.cols[:, c0:c0 + off], pst[:, :off]), r=[pk], w=["cols"])
        self.ncol += off
        assert self.ncol <= 640

    def newcols(self, name, n):
        self.colmap[name] = self.ncol
        self.ncol += n
        assert self.ncol <= 640
        return self.cols[:, self.colmap[name]:self.colmap[name] + n]

    def setup_even(self, l, L):
        E = self.E
        sfx = "%d" % l
        L["win"] = self.dram_in("ev_w_in" + sfx, [D, EV_COLS])
        L["wout"] = self.dram_in("ev_w_out" + sfx, [D, D])
        L["win_bf"] = self.dram_tmp("ev_win_bf" + sfx, [D, EV_COLS])
        L["wout_bf"] = self.dram_tmp("ev_wout_bf" + sfx, [D, D])
        self.cast_flat(L["win_bf"].ap(), L["win"].ap(), "win" + sfx)
        self.cast_flat(L["wout_bf"].ap(), L["wout"].ap(), "wout" + sfx)
        d = {}
        for nm, shp in [("lru_conv_w", [4 * 512]), ("lru_conv_b", [512]), ("lru_ba", [512]), ("lru_bx", [512]),
                        ("lru_lambda", [512]), ("rwkv_mu", [1792]), ("rwkv_w0", [512]), ("rwkv_a0", [512]),
                        ("rwkv_kk", [512]), ("rwkv_ka", [512]), ("rwkv_rk", [512]), ("rwkv_lnw", [512]),
                        ("rwkv_lnb", [512]), ("lru_wa", [8, 64, 64]), ("lru_wx", [8, 64, 64]),
                        ("rwkv_w2", [64, 512]), ("rwkv_a2", [64, 512]), ("rwkv_g2", [128, 512])]:
            d[nm] = self.dram_in(nm + sfx, shp)
        rows = lambda t, n: t.ap().rearrange("(k p) -> k p", p=P)
        items = [("convw" + sfx, rows(d["lru_conv_w"], 16), 16), ("convb" + sfx, rows(d["lru_conv_b"], 4), 4),
                 ("ba" + sfx, rows(d["lru_ba"], 4), 4), ("bx" + sfx, rows(d["lru_bx"], 4), 4),
                 ("lam" + sfx, rows(d["lru_lambda"], 4), 4), ("mu" + sfx, rows(d["rwkv_mu"], 14), 14),
                 ("w0" + sfx, rows(d["rwkv_w0"], 4), 4), ("a0" + sfx, rows(d["rwkv_a0"], 4), 4),
                 ("kk" + sfx, rows(d["rwkv_kk"], 4), 4), ("ka" + sfx, rows(d["rwkv_ka"], 4), 4),
                 ("rk" + sfx, rows(d["rwkv_rk"], 4), 4)]
        self.load_cols_multi(items)
        c8 = self.newcols("c8" + sfx, 4)
        c16 = self.newcols("c16" + sfx, 4)
        omka = self.newcols("omka" + sfx, 4)
        nw0 = self.newcols("nw0" + sfx, 4)
        omu = self.newcols("omu" + sfx, 14)
        lam = self.cols[:, self.colmap["lam" + sfx]:self.colmap["lam" + sfx] + 4]
        ka = self.cols[:, self.colmap["ka" + sfx]:self.colmap["ka" + sfx] + 4]
        w0 = self.cols[:, self.colmap["w0" + sfx]:self.colmap["w0" + sfx] + 4]
        mu = self.cols[:, self.colmap["mu" + sfx]:self.colmap["mu" + sfx] + 14]
        E("act", lambda e: e.activation(c8, lam, AF.Exp, scale=-1.0), r=["cols"], w=["cols"])
        E("act", lambda e: e.activation(c8, c8, AF.Ln, bias=self.epsc[:, 1:2]), r=["cols", "epsc"], w=["cols"])
        E("dve", lambda e: e.tensor_scalar(c16, c8, -16.0, None, ALU.mult), r=["cols"], w=["cols"])
        E("dve", lambda e: e.tensor_scalar(c8, c8, -8.0, None, ALU.mult), r=["cols"], w=["cols"])
        E("dve", lambda e: e.tensor_scalar(omka, ka, -1.0, 1.0, ALU.mult, ALU.add), r=["cols"], w=["cols"])
        E("dve", lambda e: e.tensor_scalar(nw0, w0, -1.0, None, ALU.mult), r=["cols"], w=["cols"])
        E("dve", lambda e: e.tensor_scalar(omu, mu, -1.0, 1.0, ALU.mult, ALU.add), r=["cols"], w=["cols"])
        L["lnw"] = self.sb("lnw" + sfx, [P, 512], F32)
        L["lnb"] = self.sb("lnb" + sfx, [P, 512], F32)
        for nm in ("lnw", "lnb"):
            src = d["rwkv_" + nm].ap().partition_broadcast(P)
            self.S.op("sp", lambda e, nm=nm, src=src: e.dma_start(out=L[nm][:, :], in_=src), w=[nm + sfx], dma=nm + sfx)
        L["wabd"] = self.sb("wabd" + sfx, [P, 4, P], BF16)
        L["wxbd"] = self.sb("wxbd" + sfx, [P, 4, P], BF16)
        L["w2a2"] = self.sb("w2a2" + sfx, [P, 512], BF16)
        L["g2bf"] = self.sb("g2bf" + sfx, [P, 512], BF16)
        st, stk = self.fs(8)
        for nm, dst in (("lru_wa", "wabd"), ("lru_wx", "wxbd")):
            E("pool", lambda e: e.memset(st, 0.0), w=[stk])
            for n in range(8):
                c, nl = n // 2, n % 2
                self.S.op("sp", lambda e, n=n, c=c, nl=nl, nm=nm: e.dma_start(
                    out=st[nl * 64:(nl + 1) * 64, c * P + nl * 64:c * P + (nl + 1) * 64], in_=d[nm].ap()[n]),
                    w=[stk], dma="stg8")
            E("dve", lambda e, dst=dst: e.tensor_copy(L[dst][:, :, :].rearrange("p c q -> p (c q)"), st), r=[stk], w=[dst + sfx])
        self.S.op("sp", lambda e: e.dma_start(out=st[0:64, :], in_=d["rwkv_w2"].ap()), w=[stk], dma="stg8")
        self.S.op("sp", lambda e: e.dma_start(out=st[64:128, :], in_=d["rwkv_a2"].ap()), w=[stk], dma="stg8")
        E("dve", lambda e: e.tensor_copy(L["w2a2"][:, :], st), r=[stk], w=["w2a2" + sfx])
        self.S.op("sp", lambda e: e.dma_start(out=st, in_=d["rwkv_g2"].ap()), w=[stk], dma="stg8")
        E("dve", lambda e: e.tensor_copy(L["g2bf"][:, :], st), r=[stk], w=["g2bf" + sfx])
        L["hsel"] = self.sb("hsel" + sfx, [P, 4, 8], BF16)
        E("pool", lambda e: e.memset(L["hsel"][:, :, :], 0.0), w=["hsel" + sfx])
        for c in range(4):
            for nl in range(2):
                E("pool", lambda e, c=c, nl=nl: e.memset(L["hsel"][nl * 64:(nl + 1) * 64, c, 2 * c + nl:2 * c + nl + 1], 1.0),
                  w=["hsel" + sfx])
        L["lru_h"] = self.sb("lru_h" + sfx, [P, 4], F32)
        L["lru_cc"] = self.sb("lru_cc" + sfx, [P, 4, 3], F32)
        L["sh_c"] = self.sb("sh_c" + sfx, [P, 14], F32)
        L["H"] = self.sb("H" + sfx, [P, 4, 64], F32)
        L["Hbf"] = self.sb("Hbf" + sfx, [P, 4, 64], BF16)
        for nm in ("lru_h", "lru_cc", "sh_c", "H", "Hbf"):
            t = L[nm]
            ap = t[:, :] if len(t.shape) == 2 else t[:, :, :]
            E("pool", lambda e, ap=ap: e.memset(ap, 0.0), w=[nm + sfx])

    def even_layer(self, l):
        E = self.E
        L = self.L[l]
        sfx = "%d" % l
        ti = self.ti
        self.rmsnorm_to_h("ng1_" + sfx)
        C = lambda nm, i=0: self.col(nm + sfx, i)
        tmpc, tck = self.tmpc, "tmpc"
        w_rec, wk_rec = self.get_w()
        hs = [self.fs(8 + c) for c in range(4)]
        for c in range(4):
            pst, pk = self.proj_fm(w_rec, wk_rec, c * P)
            E("pool", lambda e, c=c: e.tensor_copy(tmpc[:, 0:3], L["lru_cc"][:, c, :]), r=["lru_cc" + sfx], w=[tck])
            E("act", lambda e, pst=pst: e.activation(tmpc[:, 3:515], pst[:, :], AF.Copy), r=[pk], w=[tck])
            E("pool", lambda e, c=c: e.tensor_copy(L["lru_cc"][:, c, :], tmpc[:, 512:515]), r=[tck], w=["lru_cc" + sfx])
            xc, xck = self.fs(0)
            E("dve", lambda e, c=c: e.tensor_scalar(xc, tmpc[:, 0:512], C("convw", 0 * 4 + c), C("convb", c), ALU.mult, ALU.add),
              r=[tck, "cols"], w=[xck])
            for k in range(1, 4):
                E("dve", lambda e, c=c, k=k: e.scalar_tensor_tensor(xc, tmpc[:, k:k + 512], C("convw", k * 4 + c), xc, ALU.mult, ALU.add),
                  r=[tck, "cols", xck], w=[xck])
            xcb, xcbk = self.bs(0)
            E("pool", lambda e: e.tensor_copy(xcb, xc), r=[xck], w=[xcbk])
            pr, prk = self.psum()
            E("pe", lambda e, c=c, pr=pr: e.matmul(pr[:, :], L["wabd"][:, c, :], xcb, start=True, stop=True), r=["wabd" + sfx, xcbk], w=[prk])
            pi, pik = self.psum()
            E("pe", lambda e, c=c, pi=pi: e.matmul(pi[:, :], L["wxbd"][:, c, :], xcb, start=True, stop=True), r=["wxbd" + sfx, xcbk], w=[pik])
            rg, rgk = self.fs(1)
            ig, igk = self.fs(2)
            E("act", lambda e, c=c, pr=pr: e.activation(rg, pr[:, :], AF.Sigmoid, bias=C("ba", c)), r=[prk, "cols"], w=[rgk])
            E("act", lambda e, c=c, pi=pi: e.activation(ig, pi[:, :], AF.Sigmoid, bias=C("bx", c)), r=[pik, "cols"], w=[igk])
            av, avk = self.fs(3)
            a2, a2k = self.fs(4)
            E("act", lambda e, c=c: e.activation(av, rg, AF.Exp, scale=C("c8", c)), r=[rgk, "cols"], w=[avk])
            E("act", lambda e, c=c: e.activation(a2, rg, AF.Exp, scale=C("c16", c)), r=[rgk, "cols"], w=[a2k])
            E("dve", lambda e: e.tensor_scalar(a2, a2, -1.0, 1.0, ALU.mult, ALU.add), r=[a2k], w=[a2k])
            E("act", lambda e: e.activation(a2, a2, AF.Sqrt), r=[a2k], w=[a2k])
            if ti == 0:
                E("pool", lambda e: e.memset(a2[:, 0:1], 1.0), r=[a2k], w=[a2k])
            E("pool", lambda e: e.tensor_tensor(ig, ig, xc, ALU.mult), r=[igk, xck], w=[igk])
            E("dve", lambda e: e.tensor_tensor(ig, ig, a2, ALU.mult), r=[igk, a2k], w=[igk])
            h, hk = hs[c]
            E("dve", lambda e, c=c, h=h: e.tensor_tensor_scan(h, av, ig, L["lru_h"][:, c:c + 1], ALU.mult, ALU.add),
              r=[avk, igk, "lru_h" + sfx], w=[hk])
            E("pool", lambda e, c=c, h=h: e.tensor_copy(L["lru_h"][:, c:c + 1], h[:, 511:512]), r=[hk], w=["lru_h" + sfx])
        w_gate, wk_gate = self.get_w()
        for c in range(4):
            pst, pk = self.proj_fm(w_gate, wk_gate, c * P)
            gs, gsk = self.fs(0)
            g2_, g2k = self.fs(1)
            E("act", lambda e, pst=pst: e.activation(gs, pst[:, :], AF.Copy), r=[pk], w=[gsk])
            E("act", lambda e, pst=pst: e.activation(g2_, pst[:, :], AF.Square), r=[pk], w=[g2k])
            E("dve", lambda e: e.tensor_scalar(g2_, g2_, 0.044715, 1.0, ALU.mult, ALU.add), r=[g2k], w=[g2k])
            E("dve", lambda e: e.tensor_tensor(g2_, g2_, gs, ALU.mult), r=[g2k, gsk], w=[g2k])
            E("act", lambda e: e.activation(g2_, g2_, AF.Sigmoid, scale=1.5957691216057308), r=[g2k], w=[g2k])
            h, hk = hs[c]
            E("pool", lambda e, h=h: e.tensor_tensor(gs, gs, h, ALU.mult), r=[gsk, hk], w=[gsk])
            E("dve", lambda e, c=c: e.tensor_tensor(self.ymT[:, c, :], gs, g2_, ALU.mult), r=[gsk, g2k], w=["ymT.%d" % c])
        self.rwkv(l)
        self.out_proj_residual()

    def shift_mix(self, l, pst, pk, idx, dst, dstk):
        E = self.E
        L = self.L[l]
        sfx = "%d" % l
        tmpc, tck = self.tmpc, "tmpc"
        E("pool", lambda e: e.tensor_copy(tmpc[:, 0:1], L["sh_c"][:, idx:idx + 1]), r=["sh_c" + sfx], w=[tck])
        E("act", lambda e: e.activation(tmpc[:, 1:513], pst[:, :], AF.Copy), r=[pk], w=[tck])
        E("pool", lambda e: e.tensor_copy(L["sh_c"][:, idx:idx + 1], tmpc[:, 512:513]), r=[tck], w=["sh_c" + sfx])
        E("dve", lambda e: e.tensor_scalar(dst, tmpc[:, 0:512], self.col("mu" + sfx, idx), None, ALU.mult), r=[tck, "cols"], w=[dstk])
        E("dve", lambda e: e.scalar_tensor_tensor(dst, tmpc[:, 1:513], self.col("omu" + sfx, idx), dst, ALU.mult, ALU.add),
          r=[tck, "cols", dstk], w=[dstk])

    def rwkv(self, l):
        E = self.E
        L = self.L[l]
        sfx = "%d" % l
        C = lambda nm, i=0: self.col(nm + sfx, i)
        w_s, wk_s = self.get_w()
        xwa, xwak = self.fs(0)
        pst, pk = self.proj_fm(w_s, wk_s, 0)
        self.shift_mix(l, pst, pk, 12, xwa, xwak)
        xg, xgk = self.fs(1)
        pst, pk = self.proj_fm(w_s, wk_s, 128)
        self.shift_mix(l, pst, pk, 13, xg, xgk)
        twa, twak = self.bs(0)
        E("act", lambda e: e.activation(twa[0:64, :], xwa[0:64, :], AF.Tanh), r=[xwak], w=[twak])
        E("act", lambda e: e.activation(twa[64:128, :], xwa[64:128, :], AF.Copy), r=[xwak], w=[twak])
        sg, sgk = self.bs(1)
        E("act", lambda e: e.activation(sg, xg, AF.Sigmoid), r=[xgk], w=[sgk])
        rT = [self.fs(2 + c) for c in range(4)]
        kT = [self.fs(8 + c) for c in range(4)]
        vB = [self.bs(2 + c) for c in range(4)]
        for kind, dsts, base in (("r", rT, 0), ("k", kT, 4)):
            w, wk = self.get_w()
            for c in range(4):
                pst, pk = self.proj_fm(w, wk, c * P)
                self.shift_mix(l, pst, pk, base + c, dsts[c][0], dsts[c][1])
        w, wk = self.get_w()
        vtmp, vtk = self.fs(6)
        for c in range(4):
            pst, pk = self.proj_fm(w, wk, c * P)
            self.shift_mix(l, pst, pk, 8 + c, vtmp, vtk)
            E("act", lambda e, c=c: e.activation(vB[c][0], vtmp, AF.Copy), r=[vtk], w=[vB[c][1]])
        KA = [self.bs(6 + c) for c in range(4)]
        KT = [self.bs(10 + c) for c in range(4)]
        BT = [self.bs(14 + c) for c in range(4)]
        RT = [self.bs(18 + c) for c in range(4)]
        RK = [self.bs(22 + c) for c in range(4)]
        PC = self.fs(7)
        for c in range(4):
            t0, t0k = self.fs(6)
            t1, t1k = self.fs(0)
            t2, t2k = self.fs(1)
            p1, p1k = self.psum()
            E("pe", lambda e, c=c, p1=p1: e.matmul(p1[:, :], L["w2a2"][0:64, c * P:(c + 1) * P], twa[0:64, :], start=True, stop=True),
              r=["w2a2" + sfx, twak], w=[p1k])
            E("act", lambda e, c=c, p1=p1: e.activation(t0, p1[:, :], AF.Exp, bias=C("nw0", c), scale=-1.0), r=[p1k, "cols"], w=[t0k])
            E("act", lambda e: e.activation(t0, t0, AF.Ln, bias=self.epsc[:, 1:2]), r=[t0k, "epsc"], w=[t0k])
            E("act", lambda e: e.activation(t0, t0, AF.Exp, bias=self.epsc[:, 2:3], scale=-1.0), r=[t0k, "epsc"], w=[t0k])
            for j in range(4):
                sl = slice(j * P, (j + 1) * P)
                E("dve", lambda e, sl=sl: e.tensor_tensor_scan(t1[:, sl], self.ones32[:, 0:P], t0[:, sl], 0.0, ALU.mult, ALU.add),
                  r=[t0k, "ones32"], w=[t1k])
            pinv, pinvk = self.fs(6)
            pprev, pprevk = t2, t2k
            E("dve", lambda e: e.tensor_tensor(t2, t0, t1, ALU.subtract), r=[t0k, t1k], w=[t2k])
            E("act", lambda e: e.activation(t2, t2, AF.Exp), r=[t2k], w=[t2k])
            E("act", lambda e: e.activation(t0, t1, AF.Exp), r=[t1k], w=[t0k])
            E("act", lambda e: e.activation(t1, t1, AF.Exp, scale=-1.0), r=[t1k], w=[t1k])
            E("dve", lambda e, c=c: e.tensor_tensor(RT[c][0], rT[c][0], t1, ALU.mult), r=[rT[c][1], t1k], w=[RT[c][1]])
            for j in range(4):
                E("pool", lambda e, c=c, j=j: e.tensor_copy(PC[0][:, c * 4 + j:c * 4 + j + 1], t1[:, j * P + P - 1:j * P + P]),
                  r=[t1k], w=[PC[1]])
            p2, p2k = self.psum()
            E("pe", lambda e, c=c, p2=p2: e.matmul(p2[:, :], L["w2a2"][64:128, c * P:(c + 1) * P], twa[64:128, :], start=True, stop=True),
              r=["w2a2" + sfx, twak], w=[p2k])
            av, avk = t1, t1k
            E("act", lambda e, c=c, p2=p2: e.activation(av, p2[:, :], AF.Sigmoid, bias=C("a0", c)), r=[p2k, "cols", RT[c][1], PC[1]], w=[avk])
            kr, krk = self.fs(7 + 0) if False else (self.tmpc[:, 0:512], "tmpc")
            E("dve", lambda e, c=c: e.tensor_scalar(kr, kT[c][0], C("kk", c), None, ALU.mult), r=[kT[c][1], "cols"], w=[krk])
            sqb, sqbk = self.tmpb[0], "tmpb0"
            E("act", lambda e: e.activation(sqb[:, :], kr, AF.Square), r=[krk], w=[sqbk])
            p3, p3k = self.psum()
            E("pe", lambda e, p3=p3: e.matmul(p3[:, :], self.ones_blk[:, :], sqb[:, :], start=True, stop=True), r=["ones_blk", sqbk], w=[p3k])
            rs, rsk = self.rstd, "rstd"
            E("act", lambda e, p3=p3: e.activation(rs[:, :], p3[:, :], AF.Sqrt, bias=self.epsc[:, 0:1]), r=[p3k, "epsc"], w=[rsk])
            E("dve", lambda e: e.reciprocal(rs[:, :], rs[:, :]), r=[rsk], w=[rsk])
            E("dve", lambda e: e.tensor_tensor(kr, kr, rs[:, :], ALU.mult), r=[krk, rsk], w=[krk])
            E("dve", lambda e, c=c: e.tensor_tensor(KA[c][0], kr, t2, ALU.mult), r=[krk, t2k], w=[KA[c][1]])
            E("pool", lambda e: e.tensor_tensor(kr, kr, av, ALU.mult), r=[krk, avk], w=[krk])
            E("dve", lambda e, c=c: e.tensor_tensor(BT[c][0], kr, t0, ALU.mult), r=[krk, t0k], w=[BT[c][1]])
            E("dve", lambda e, c=c: e.tensor_scalar(av, av, C("ka", c), C("omka", c), ALU.mult, ALU.add), r=[avk, "cols"], w=[avk])
            E("pool", lambda e, c=c: e.tensor_tensor(av, av, kT[c][0], ALU.mult), r=[avk, kT[c][1]], w=[avk])
            E("dve", lambda e, c=c: e.tensor_tensor(KT[c][0], av, t0, ALU.mult), r=[avk, t0k], w=[KT[c][1]])
            E("pool", lambda e, c=c: e.tensor_tensor(av, av, rT[c][0], ALU.mult), r=[avk, rT[c][1]], w=[avk])
            E("dve", lambda e, c=c: e.tensor_scalar(RK[c][0], av, C("rk", c), None, ALU.mult), r=[avk, "cols"], w=[RK[c][1]])
        for nm, lst in (("KA", KA), ("KT", KT), ("BT", BT), ("RT", RT), ("vB", vB), ("RK", RK)):
            self.tap(nm + "0", lst[0][0], lst[0][1], [P, 512], BF16)
        G = [self.bs(26), self.bs(27)]
        scr = [self.bs(26 + i) for i in range(6)] + [(self.tmpb[1][:, :], "tmpb1"), (self.tmpb[2][:, :], "tmpb2")]
        GT = self.sb("rw_G" + sfx + "_%d" % self.ti, [P, 8, 512], BF16) if not hasattr(self, "rw_G") else self.rw_G
        self.rw_G = GT
        if not hasattr(self, "rw_tok"):
            self.rw_tok = self.sb("rw_tok", [P, 3, 512], BF16)
            self.rw_X = self.sb("rw_X", [P, 8, 64], BF16)
            self.rw_U = self.sb("rw_U", [P, 8, 64], BF16)
            self.rw_N0 = self.sb("rw_N0", [P, 2, 512], BF16)
            self.rw_st = self.sb("rw_st", [P, 64], F32)
        tok = self.rw_tok
        for j in range(4):
            bl = slice(j * P, (j + 1) * P)
            for q, srcs in enumerate((KT, BT, vB)):
                pb, pbk = self.psumb()
                for c in range(4):
                    E("pe", lambda e, c=c, srcs=srcs, pb=pb: e.transpose(pb[:, c * P:(c + 1) * P], srcs[c][0][:, bl], self.ident_bf[:, 0, :]),
                      r=[srcs[c][1], "ident_bf"], w=[pbk])
                E("act", lambda e, q=q, pb=pb: e.activation(tok[:, q, :], pb[:, 0:512], AF.Copy), r=[pbk], w=["rw_tok%d" % q])
            for hd in range(8):
                c, hh = hd // 2, hd % 2
                ps_ = slice(hh * 64, (hh + 1) * 64)
                pg, pgk = self.psum()
                for q, (lh, rh) in enumerate(((BT, KA), (KT, KA), (KT, RT), (BT, RT))):
                    E("pe", lambda e, q=q, lh=lh, rh=rh, c=c, pg=pg: e.matmul(pg[:, q * P:(q + 1) * P], lh[c][0][ps_, bl], rh[c][0][ps_, bl], start=True, stop=True),
                      r=[lh[c][1], rh[c][1]], w=[pgk])
                E("dve", lambda e, hd=hd, pg=pg: e.tensor_tensor(GT[:, hd, :], pg[:, :], self.mask4[:, :, :].rearrange("p g c -> p (g c)"), ALU.mult),
                  r=[pgk, "mask4"], w=["rw_G%d" % hd])
                E("pool", lambda e, hd=hd: e.tensor_copy(self.rw_N0[:, hd // 4, (hd % 4) * P:(hd % 4 + 1) * P], GT[:, hd, 0:P]),
                  r=["rw_G%d" % hd], w=["rw_N0_%d" % (hd // 4)])
            self.tap("G0", GT[:, 0, :], "rw_G0", [P, 512], BF16)
            self.tap("tok", tok[:, :, :], "rw_tok2", [P, 3, 512], BF16)
            Ws = []
            for grp in range(2):
                scr_g = scr
                W, Wk = self.tri_inverse(self.rw_N0[:, grp, :], "rw_N0_%d" % grp, 4, scr_g)
                dstW = self.rw_N0[:, grp, :]
                E("pool", lambda e, W=W, dstW=dstW: e.tensor_copy(dstW, W[:, 0:512]), r=[Wk], w=["rw_N0_%d" % grp])
                Ws.append((dstW, "rw_N0_%d" % grp))
            self.tap("W0", Ws[0][0], Ws[0][1], [P, 512], BF16)
            py, pyk = self.psum_long()
            for hd in range(8):
                c, hh = hd // 2, hd % 2
                ps_ = slice(hh * 64, (hh + 1) * 64)
                vt = tok[:, 2, hd * 64:(hd + 1) * 64]
                W = Ws[hd // 4][0][:, (hd % 4) * P:(hd % 4 + 1) * P]
                Wk = Ws[hd // 4][1]
                px, pxk = self.psum()
                E("pe", lambda e, c=c, px=px: e.matmul(px[:, 0:64], KA[c][0][ps_, bl], L["Hbf"][ps_, c, :], start=True, stop=False),
                  r=[KA[c][1], "Hbf" + sfx], w=[pxk])
                E("pe", lambda e, hd=hd, px=px, vt=vt: e.matmul(px[:, 0:64], GT[:, hd, P:2 * P], vt, start=False, stop=True),
                  r=["rw_G%d" % hd, "rw_tok2"], w=[pxk])
                E("act", lambda e, hd=hd, px=px: e.activation(self.rw_X[:, hd, :], px[:, 0:64], AF.Copy), r=[pxk], w=["rw_X%d" % hd])
                pu, puk = self.psum()
                E("pe", lambda e, hd=hd, pu=pu, W=W: e.matmul(pu[:, 0:64], W, self.rw_X[:, hd, :], start=True, stop=True),
                  r=[Wk, "rw_X%d" % hd], w=[puk])
                E("act", lambda e, hd=hd, pu=pu: e.activation(self.rw_U[:, hd, :], pu[:, 0:64], AF.Copy, scale=-1.0), r=[puk], w=["rw_U%d" % hd])
                ysl = slice(hd * 64, (hd + 1) * 64)
                E("pe", lambda e, c=c, ysl=ysl: e.matmul(py[:, ysl], RT[c][0][ps_, bl], L["Hbf"][ps_, c, :], start=True, stop=False),
                  r=[RT[c][1], "Hbf" + sfx], w=[pyk])
                E("pe", lambda e, hd=hd, ysl=ysl, vt=vt: e.matmul(py[:, ysl], GT[:, hd, 2 * P:3 * P], vt, start=False, stop=False),
                  r=["rw_G%d" % hd, "rw_tok2"], w=[pyk])
                E("pe", lambda e, hd=hd, ysl=ysl: e.matmul(py[:, ysl], GT[:, hd, 3 * P:4 * P], self.rw_U[:, hd, :], start=False, stop=True),
                  r=["rw_G%d" % hd, "rw_U%d" % hd], w=[pyk])
                ph, phk = self.psum()
                E("pe", lambda e, c=c, ph=ph, vt=vt: e.matmul(ph[:, 0:64], tok[:, 0, c * P:(c + 1) * P], vt, start=True, stop=False),
                  r=["rw_tok0", "rw_tok2"], w=[phk])
                E("pe", lambda e, c=c, ph=ph, hd=hd: e.matmul(ph[:, 0:64], tok[:, 1, c * P:(c + 1) * P], self.rw_U[:, hd, :], start=False, stop=True),
                  r=["rw_tok1", "rw_U%d" % hd], w=[phk])
                E("dve", lambda e, c=c, ph=ph: e.tensor_tensor(self.rw_st[ps_, :], ph[ps_, 0:64], L["H"][ps_, c, :], ALU.add),
                  r=[phk, "H" + sfx], w=["rw_st"])
                E("dve", lambda e, c=c, j=j: e.tensor_scalar(L["H"][ps_, c, :], self.rw_st[ps_, :], PC[0][ps_, c * 4 + j:c * 4 + j + 1], None, ALU.mult),
                  r=["rw_st", PC[1]], w=["H" + sfx])
                E("pool", lambda e, c=c: e.tensor_copy(L["Hbf"][ps_, c, :], L["H"][ps_, c, :]), r=["H" + sfx], w=["Hbf" + sfx])
            self.tap("X", self.rw_X[:, 0, :], "rw_X0", [P, 64], BF16)
            self.tap("U", self.rw_U[:, 0, :], "rw_U0", [P, 64], BF16)
            self.rwkv_post(l, j, py, pyk, RK, sg, sgk)

    def rwkv_post(self, l, j, py, pyk, RK, sg, sgk):
        E = self.E
        L = self.L[l]
        sfx = "%d" % l
        bl = slice(j * P, (j + 1) * P)
        tok = self.rw_tok
        Y, Yk = self.fs(0)
        Y2, Y2k = self.fs(1)
        E("act", lambda e: e.activation(Y, py[:, :], AF.Copy), r=[pyk], w=[Yk])
        self.tap("Yraw%d" % j, Y, Yk, [P, 512])
        E("act", lambda e: e.activation(Y2, py[:, :], AF.Square), r=[pyk], w=[Y2k])
        st = self.rstd
        stk = "rstd"
        E("dve", lambda e: e.tensor_reduce(st[:, 0:8], Y.rearrange("p (h v) -> p h v", v=64), AX.X, ALU.add), r=[Yk], w=[stk])
        E("dve", lambda e: e.tensor_reduce(st[:, 8:16], Y2.rearrange("p (h v) -> p h v", v=64), AX.X, ALU.add), r=[Y2k, stk], w=[stk])
        E("dve", lambda e: e.tensor_scalar(st[:, 16:24], st[:, 0:8], 1.0 / 64, None, ALU.mult), r=[stk], w=[stk])
        E("dve", lambda e: e.tensor_tensor(st[:, 0:8], st[:, 16:24], st[:, 16:24], ALU.mult), r=[stk], w=[stk])
        E("dve", lambda e: e.scalar_tensor_tensor(st[:, 24:32], st[:, 8:16], 1.0 / 64, st[:, 0:8], ALU.mult, ALU.subtract),
          r=[stk], w=[stk])
        E("act", lambda e: e.activation(st[:, 24:32], st[:, 24:32], AF.Sqrt, bias=self.epsc[:, 3:4]), r=[stk, "epsc"], w=[stk])
        E("dve", lambda e: e.reciprocal(st[:, 24:32], st[:, 24:32]), r=[stk], w=[stk])
        Y3 = Y.rearrange("p (h v) -> p h v", v=64)
        E("dve", lambda e: e.tensor_tensor(Y3, Y3, st[:, 16:24].unsqueeze(2).to_broadcast([P, 8, 64]), ALU.subtract), r=[Yk, stk], w=[Yk])
        E("dve", lambda e: e.tensor_tensor(Y3, Y3, st[:, 24:32].unsqueeze(2).to_broadcast([P, 8, 64]), ALU.mult), r=[Yk, stk], w=[Yk])
        E("pool", lambda e: e.tensor_tensor(Y, Y, L["lnw"][:, :], ALU.mult), r=[Yk, "lnw" + sfx], w=[Yk])
        E("pool", lambda e: e.tensor_tensor(Y, Y, L["lnb"][:, :], ALU.add), r=[Yk, "lnb" + sfx], w=[Yk])
        pbn, pbnk = self.psum()
        for c in range(4):
            E("pe", lambda e, c=c: e.matmul(pbn[:, 0:8], RK[c][0][:, bl], L["hsel"][:, c, :], start=(c == 0), stop=(c == 3)),
              r=[RK[c][1], "hsel" + sfx], w=[pbnk])
        E("act", lambda e: e.activation(st[:, 32:40], pbn[:, 0:8], AF.Copy), r=[pbnk, stk], w=[stk])
        E("dve", lambda e: e.tensor_tensor(Y2.rearrange("p (h v) -> p h v", v=64), tok[:, 2, :].rearrange("p (h v) -> p h v", v=64),
                                           st[:, 32:40].unsqueeze(2).to_broadcast([P, 8, 64]), ALU.mult), r=["rw_tok2", stk, Y2k], w=[Y2k])
        E("pool", lambda e: e.tensor_tensor(Y, Y, Y2, ALU.add), r=[Yk, Y2k], w=[Yk])
        pgt, pgtk = self.psum()
        E("pe", lambda e: e.matmul(pgt[:, :], sg[:, bl], L["g2bf"][:, :], start=True, stop=True), r=[sgk, "g2bf" + sfx], w=[pgtk])
        yb, ybk = self.tmpb[0], "tmpb0"
        E("dve", lambda e: e.tensor_tensor(yb[:, :], pgt[:, :], Y, ALU.mult), r=[pgtk, Yk], w=[ybk])
        pb, pbk = self.psumb()
        for c in range(4):
            E("pe", lambda e, c=c: e.transpose(pb[:, c * P:(c + 1) * P], yb[:, c * P:(c + 1) * P], self.ident_bf[:, 0, :]),
              r=[ybk, "ident_bf"], w=[pbk])
        E("act", lambda e: e.activation(self.ymT[:, 4:8, bl], pb[:, 0:512].rearrange("p (c t) -> p c t", t=P), AF.Copy),
          r=[pbk], w=["ymT.%d" % c for c in range(4, 8)])

    def bcast_load(self, name, src1d, n, reps=1):
        t = self.sb(name, [P, n * reps], F32)
        for r_ in range(reps):
            self.S.op("sp", lambda e, r_=r_: e.dma_start(out=t[:, r_ * n:(r_ + 1) * n], in_=src1d.partition_broadcast(P)),
                      w=[name], dma="bc_" + name)
        return t

    def setup_odd(self, l, L):
        E = self.E
        sfx = "%d" % l
        L["win"] = self.dram_in("od_w_in" + sfx, [D, OD_COLS])
        L["wout"] = self.dram_in("od_w_out" + sfx, [D, D])
        L["win_bf"] = self.dram_tmp("od_win_bf" + sfx, [D, OD_COLS])
        L["wout_bf"] = self.dram_tmp("od_wout_bf" + sfx, [D, D])
        L["gw_bf"] = self.dram_tmp("od_gw_bf" + sfx, [D, 16])
        self.cast_flat(L["win_bf"].ap(), L["win"].ap(), "win" + sfx)
        self.cast_flat(L["wout_bf"].ap(), L["wout"].ap(), "wout" + sfx)
        self.cast_dram(L["gw_bf"].ap()[:, 0:8], L["win"].ap()[:, 2048:2056], "gwa" + sfx)
        self.cast_dram(L["gw_bf"].ap()[:, 8:16], L["win"].ap()[:, 3592:3600], "gwb" + sfx)
        d = {}
        for nm, shp in [("gdn_conv_w", [4 * 1536]), ("gdn_a_log", [4]), ("gdn_dt_bias", [4]), ("gdn_norm_g", [128]),
                        ("mlstm_conv_w", [4 * 512]), ("mlstm_ig_b", [4]), ("mlstm_fg_b", [4]), ("mlstm_norm_g", [128])]:
            d[nm] = self.dram_in(nm + sfx, shp)
        rows = lambda t: t.ap().rearrange("(k p) -> k p", p=P)
        self.load_cols_multi([("gconvw" + sfx, rows(d["gdn_conv_w"]), 48), ("mconvw" + sfx, rows(d["mlstm_conv_w"]), 16)])
        L["dtb"] = self.bcast_load("dtb" + sfx, d["gdn_dt_bias"].ap(), 4)
        L["negA"] = self.bcast_load("negA" + sfx, d["gdn_a_log"].ap(), 4)
        L["igb"] = self.bcast_load("igb" + sfx, d["mlstm_ig_b"].ap(), 4)
        L["fgb"] = self.bcast_load("fgb" + sfx, d["mlstm_fg_b"].ap(), 4)
        L["ngg"] = self.bcast_load("ngg" + sfx, d["gdn_norm_g"].ap(), 128, reps=4)
        L["ngm"] = self.bcast_load("ngm" + sfx, d["mlstm_norm_g"].ap(), 128, reps=4)
        E("act", lambda e: e.activation(L["negA"][:, :], L["negA"][:, :], AF.Exp), r=["negA" + sfx], w=["negA" + sfx])
        E("dve", lambda e: e.tensor_scalar(L["negA"][:, :], L["negA"][:, :], -1.0, None, ALU.mult), r=["negA" + sfx], w=["negA" + sfx])
        L["g_cc"] = self.sb("g_cc" + sfx, [P, 12, 3], F32)
        L["m_cc"] = self.sb("m_cc" + sfx, [P, 4, 3], F32)
        L["S"] = self.sb("gS" + sfx, [P, 4, P], F32)
        L["Sbf"] = self.sb("gSbf" + sfx, [P, 4, P], BF16)
        L["C"] = self.sb("mC" + sfx, [P, 2, 130], F32)
        L["Cbf"] = self.sb("mCbf" + sfx, [P, 2, 130], BF16)
        for nm in ("g_cc", "m_cc", "S", "Sbf", "C", "Cbf"):
            key = {"S": "gS", "Sbf": "gSbf", "C": "mC", "Cbf": "mCbf"}.get(nm, nm) + sfx
            E("pool", lambda e, nm=nm: e.memset(L[nm][:, :, :], 0.0), w=[key])
        if not hasattr(self, "od_vaug"):
            self.od_vaug = self.sb("od_vaug", [P, 4, 4, 130], BF16)
            self.od_gt = self.sb("od_gt", [P, 4, 16], F32)
            self.od_sm = self.sb("od_sm", [P, 12, 16], F32)
            self.od_cum = self.sb("od_cum", [P, 32], F32)
            self.od_wk = self.sb("od_wk", [P, 4, 64], BF16)
            self.od_qd = self.sb("od_qd", [P, 4, P], BF16)
            self.od_qm = self.sb("od_qm", [P, 4, P], BF16)
            E("pool", lambda e: e.memset(self.od_qd[:, :, :], 0.0), w=["od_qd"])
            E("pool", lambda e: e.memset(self.od_qm[:, :, :], 0.0), w=["od_qm"])
            E("pool", lambda e: e.memset(self.od_vaug[:, :, :, :], 1.0), w=["od_vaug"])

    def plan_odd(self, l, L):
        w = L["win_bf"].ap()
        k = "win%d" % l
        self.wplan.append((L["gw_bf"].ap().rearrange("(k p) c -> p k c", p=P), 16, ("gwa%d" % l, "gwb%d" % l)))
        for c0 in (0, 512, 1024, 1536, 2056, 2568, 3080):
            self.plan_w(w, 0, c0, 512, k)
        for h in range(2):
            self.plan_w(L["wout_bf"].ap(), 0, h * 512, 512, "wout%d" % l)

    def conv_silu(self, l, pst, pk, cc, cck, idx, wname, widx_stride, dst, dstk, scale=1.0):
        E = self.E
        tmpc, tck = self.tmpc, "tmpc"
        E("pool", lambda e: e.tensor_copy(tmpc[:, 0:3], cc[:, idx, :]), r=[cck], w=[tck])
        E("act", lambda e: e.activation(tmpc[:, 3:515], pst[:, :], AF.Copy), r=[pk], w=[tck])
        E("pool", lambda e: e.tensor_copy(cc[:, idx, :], tmpc[:, 512:515]), r=[tck], w=[cck])
        xc, xck = self.fs(19)
        E("dve", lambda e: e.tensor_scalar(xc, tmpc[:, 0:512], self.col(wname, 0 * widx_stride + idx), None, ALU.mult),
          r=[tck, "cols"], w=[xck])
        for k in range(1, 4):
            E("dve", lambda e, k=k: e.scalar_tensor_tensor(xc, tmpc[:, k:k + 512], self.col(wname, k * widx_stride + idx), xc, ALU.mult, ALU.add),
              r=[tck, "cols", xck], w=[xck])
        sg, sgk = self.fs(18)
        E("act", lambda e: e.activation(sg, xc, AF.Sigmoid), r=[xck], w=[sgk])
        if scale == 1.0:
            E("dve", lambda e: e.tensor_tensor(dst, xc, sg, ALU.mult), r=[xck, sgk], w=[dstk])
        else:
            E("dve", lambda e: e.scalar_tensor_tensor(dst, xc, scale, sg, ALU.mult, ALU.mult), r=[xck, sgk], w=[dstk])

    def odd_layer(self, l):
        E = self.E
        L = self.L[l]
        sfx = "%d" % l
        self.rmsnorm_to_h("ng1_" + sfx)
        gt, sm, cum = self.od_gt, self.od_sm, self.od_cum
        SM = lambda i: sm[:, i, :]
        SM3 = lambda i: sm[:, i, :].rearrange("p (b h) -> p b h", h=4)
        wg, wgk = self.get_w()
        for b in range(4):
            pst, pk = self.proj_tm(wg, wgk, 0, 16, b)
            E("act", lambda e, b=b, pst=pst: e.activation(gt[:, b, :], pst[:, 0:16], AF.Copy), r=[pk], w=["od_gt"])
        bc4 = lambda t: t[:, :].unsqueeze(1).to_broadcast([P, 4, 4])
        E("act", lambda e: e.activation(SM3(0), gt[:, :, 0:4], AF.Sigmoid), r=["od_gt"], w=["od_sm0"])
        E("dve", lambda e: e.tensor_scalar(SM(1), SM(0), -1.0, None, ALU.mult), r=["od_sm0"], w=["od_sm1"])
        E("dve", lambda e: e.tensor_tensor(SM3(5), gt[:, :, 4:8], bc4(L["dtb"]), ALU.add), r=["od_gt", "dtb" + sfx], w=["od_sm5"])
        E("act", lambda e: e.activation(SM(5), SM(5), AF.Exp), r=["od_sm5"], w=["od_sm5"])
        E("act", lambda e: e.activation(SM(5), SM(5), AF.Ln, bias=self.epsc[:, 1:2]), r=["od_sm5", "epsc"], w=["od_sm5"])
        E("dve", lambda e: e.tensor_tensor(cum[:, 0:16].rearrange("p (b h) -> p b h", h=4), SM3(5), bc4(L["negA"]), ALU.mult),
          r=["od_sm5", "negA" + sfx], w=["od_cum"])
        E("dve", lambda e: e.tensor_tensor(SM3(3), gt[:, :, 8:12], bc4(L["igb"]), ALU.add), r=["od_gt", "igb" + sfx], w=["od_sm3"])
        E("dve", lambda e: e.tensor_tensor(SM3(5), gt[:, :, 12:16], bc4(L["fgb"]), ALU.add), r=["od_gt", "fgb" + sfx, "od_cum"], w=["od_sm5"])
        E("act", lambda e: e.activation(SM(5), SM(5), AF.Exp, scale=-1.0), r=["od_sm5"], w=["od_sm5"])
        E("act", lambda e: e.activation(SM(5), SM(5), AF.Ln, bias=self.epsc[:, 1:2]), r=["od_sm5", "epsc"], w=["od_sm5"])
        E("dve", lambda e: e.tensor_scalar(cum[:, 16:32], SM(5), -1.0, None, ALU.mult), r=["od_sm5"], w=["od_cum"])
        pcs, pcsk = self.psum()
        E("pe", lambda e: e.matmul(pcs[:, 0:32], self.triU[:, :], cum[:, 0:32], start=True, stop=True), r=["triU", "od_cum"], w=[pcsk])
        E("act", lambda e: e.activation(SM(6), pcs[:, 0:16], AF.Copy), r=[pcsk], w=["od_sm6"])
        E("act", lambda e: e.activation(SM(7), pcs[:, 16:32], AF.Copy), r=[pcsk], w=["od_sm7"])
        E("act", lambda e: e.activation(SM(8), pcs[:, 0:16], AF.Copy, scale=-1.0), r=[pcsk], w=["od_sm8"])
        E("act", lambda e: e.activation(SM(9), pcs[:, 16:32], AF.Copy, scale=-1.0), r=[pcsk], w=["od_sm9"])
        E("act", lambda e: e.activation(SM(10), SM(3), AF.Exp), r=["od_sm3"], w=["od_sm10"])
        skip = getattr(self, "skip", ())
        if "gdn" in skip:
            for _ in range(4):
                self.get_w()
            for c in range(4):
                self.E("pool", lambda e, c=c: e.memset(self.ymT[:, c, :], 0.0), w=["ymT.%d" % c])
        else:
            self.gdn(l)
        if "mlstm" in skip:
            for _ in range(3):
                self.get_w()
            for c in range(4, 8):
                self.E("pool", lambda e, c=c: e.memset(self.ymT[:, c, :], 0.0), w=["ymT.%d" % c])
        else:
            self.mlstm(l)
        self.out_proj_residual()

    def bcast_cum(self, smi, j):
        E = self.E
        sm = self.od_sm
        dg, dgk = self.fs(17)
        for h in range(4):
            E("dve", lambda e, h=h: e.tensor_scalar(dg[:, h * P:(h + 1) * P], self.ident[:, :], sm[:, smi, j * 4 + h:j * 4 + h + 1], None, ALU.mult),
              r=["ident", "od_sm%d" % smi], w=[dgk])
        pbc, pbck = self.psum()
        for h in range(4):
            E("pe", lambda e, h=h: e.matmul(pbc[:, h * P:(h + 1) * P], self.ones32[:, 0:P], dg[:, h * P:(h + 1) * P], start=True, stop=True),
              r=["ones32", dgk], w=[pbck])
        return pbc, pbck

    def head_rmsnorm_out(self, src, srck, ng, ngk, gate, gatek, ybase, bl):
        E = self.E
        sq, sqk = self.fs(17)
        st, stk = self.rstd, "rstd"
        E("act", lambda e: e.activation(sq, src, AF.Square), r=[srck], w=[sqk])
        E("dve", lambda e: e.tensor_reduce(st[:, 0:4], sq.rearrange("p (h v) -> p h v", v=P), AX.X, ALU.add), r=[sqk], w=[stk])
        E("act", lambda e: e.activation(st[:, 0:4], st[:, 0:4], AF.Sqrt, bias=self.epsc[:, 0:1], scale=1.0 / P), r=[stk, "epsc"], w=[stk])
        E("dve", lambda e: e.reciprocal(st[:, 0:4], st[:, 0:4]), r=[stk], w=[stk])
        s3 = src.rearrange("p (h v) -> p h v", v=P)
        E("dve", lambda e: e.tensor_tensor(s3, s3, st[:, 0:4].unsqueeze(2).to_broadcast([P, 4, P]), ALU.mult), r=[srck, stk], w=[srck])
        E("pool", lambda e: e.tensor_tensor(src, src, ng[:, :], ALU.mult), r=[srck, ngk], w=[srck])
        yb, ybk = self.tmpb[0], "tmpb0"
        E("dve", lambda e: e.tensor_tensor(yb[:, :], src, gate, ALU.mult), r=[srck, gatek], w=[ybk])
        pb, pbk = self.psumb()
        for c in range(4):
            E("pe", lambda e, c=c: e.transpose(pb[:, c * P:(c + 1) * P], yb[:, c * P:(c + 1) * P], self.ident_bf[:, 0, :]),
              r=[ybk, "ident_bf"], w=[pbk])
        E("act", lambda e: e.activation(self.ymT[:, ybase:ybase + 4, bl], pb[:, 0:512].rearrange("p (c t) -> p c t", t=P), AF.Copy),
          r=[pbk], w=["ymT.%d" % c for c in range(ybase, ybase + 4)])

    def gdn(self, l):
        E = self.E
        L = self.L[l]
        sfx = "%d" % l
        sm = self.od_sm
        qT = [self.bs(h) for h in range(4)]
        kT = [self.bs(4 + h) for h in range(4)]
        vT = [self.bs(8 + h) for h in range(4)]
        zs = [self.bs(12 + b) for b in range(4)]
        for kind, dsts in (("q", qT), ("k", kT), ("v", vT)):
            w, wk = self.get_w()
            base = {"q": 0, "k": 4, "v": 8}[kind]
            for h in range(4):
                pst, pk = self.proj_fm(w, wk, h * P)
                if kind == "v":
                    self.conv_silu(l, pst, pk, L["g_cc"], "g_cc" + sfx, base + h, "gconvw" + sfx, 12, dsts[h][0], dsts[h][1])
                else:
                    xf, xfk = self.fs(16)
                    self.conv_silu(l, pst, pk, L["g_cc"], "g_cc" + sfx, base + h, "gconvw" + sfx, 12, xf, xfk)
                    sqb, sqbk = self.tmpb[0], "tmpb0"
                    E("act", lambda e: e.activation(sqb[:, :], xf, AF.Square), r=[xfk], w=[sqbk])
                    p3, p3k = self.psum()
                    E("pe", lambda e: e.matmul(p3[:, :], self.ones_bf[:, :], sqb[:, :], start=True, stop=True), r=["ones_bf", sqbk], w=[p3k])
                    rs, rsk = self.rstd, "rstd"
                    E("act", lambda e: e.activation(rs[:, :], p3[:, :], AF.Sqrt, bias=self.epsc[:, 0:1]), r=[p3k, "epsc"], w=[rsk])
                    E("dve", lambda e: e.reciprocal(rs[:, :], rs[:, :]), r=[rsk], w=[rsk])
                    sc = (128.0 ** -0.5) if kind == "q" else 1.0
                    E("dve", lambda e: e.scalar_tensor_tensor(dsts[h][0], xf, sc, rs[:, :], ALU.mult, ALU.mult), r=[xfk, rsk], w=[dsts[h][1]])
        w, wk = self.get_w()
        for b in range(4):
            pst, pk = self.proj_tm(w, wk, 0, 512, b)
            zt, ztk = self.fs(16)
            E("act", lambda e: e.activation(zt, pst[:, :], AF.Sigmoid), r=[pk], w=[ztk])
            E("dve", lambda e: e.tensor_tensor(zs[b][0], pst[:, :], zt, ALU.mult), r=[pk, ztk], w=[zs[b][1]])
        gstop = getattr(self, "gstop", 99)
        if gstop < 99:
            for c in range(4):
                self.E("pool", lambda e, c=c: e.memset(self.ymT[:, c, :], 0.0), w=["ymT.%d" % c])
        if gstop <= 1:
            return
        scr = [self.bs(24 + i) for i in range(6)] + [self.bs(22), self.bs(23)]
        N0, N0k = self.bs(30)
        MT, MTk = self.bs(31)
        kg, kgk = self.bs(16)
        qd, qdk = self.bs(17)
        vtok, vtokk = self.bs(18)
        kdec, kdeck = self.bs(19)
        Rb, Rbk = self.bs(20)
        vn, vnk = self.bs(21)
        for j in range(4):
            bl = slice(j * P, (j + 1) * P)
            pbc, pbck = self.bcast_cum(6, j)
            if gstop <= 1.2:
                return
            Ebc, Ebck = self.fs(0)
            E("act", lambda e: e.activation(Ebc, pbc[:, :], AF.Exp), r=[pbck], w=[Ebck])
            if gstop <= 1.4:
                return
            Dm, Dmk = self.fs(1)
            for h in range(4):
                E("act", lambda e, h=h: e.activation(Dm[:, h * P:(h + 1) * P], pbc[:, h * P:(h + 1) * P], AF.Exp,
                                                     bias=sm[:, 8, j * 4 + h:j * 4 + h + 1]),
                  r=[pbck, "od_sm8"], w=[Dmk])
            if gstop <= 1.6:
                return
            E("dve", lambda e: e.tensor_scalar(Dm, Dm, 1.0, None, ALU.min), r=[Dmk], w=[Dmk])
            if gstop <= 1.8:
                return
            Ds, Dsk = self.fs(2)
            mS = self.maskS[:, :, :].rearrange("p g c -> p (g c)")
            mI = self.maskI[:, :, :].rearrange("p g c -> p (g c)")
            E("dve", lambda e: e.tensor_tensor(Ds, Dm, mS, ALU.mult), r=[Dmk, "maskS"], w=[Dsk])
            E("dve", lambda e: e.tensor_tensor(Dm, Dm, mI, ALU.mult), r=[Dmk, "maskI"], w=[Dmk])
            if gstop <= 2:
                return
            pkk, pkkk = self.psum()
            pkq, pkqk = self.psum()
            for h in range(4):
                sl = slice(h * P, (h + 1) * P)
                E("pe", lambda e, h=h, sl=sl: e.matmul(pkk[:, sl], kT[h][0][:, bl], kT[h][0][:, bl], start=True, stop=True), r=[kT[h][1]], w=[pkkk])
            for h in range(4):
                sl = slice(h * P, (h + 1) * P)
                E("pe", lambda e, h=h, sl=sl: e.matmul(pkq[:, sl], kT[h][0][:, bl], qT[h][0][:, bl], start=True, stop=True), r=[kT[h][1], qT[h][1]], w=[pkqk])
            for h in range(4):
                sl = slice(h * P, (h + 1) * P)
                E("dve", lambda e, h=h, sl=sl: e.scalar_tensor_tensor(N0[:, sl], pkk[:, sl], sm[:, 1, j * 4 + h:j * 4 + h + 1], Ds[:, sl], ALU.mult, ALU.mult),
                  r=[pkkk, "od_sm1", Dsk], w=[N0k])
            E("dve", lambda e: e.tensor_tensor(MT, pkq[:, :], Dm, ALU.mult), r=[pkqk, Dmk], w=[MTk])
            if gstop <= 3:
                return
            W, Wk = self.tri_inverse(N0, N0k, 4, scr)
            if gstop <= 4:
                return
            for h in range(4):
                sl = slice(h * P, (h + 1) * P)
                E("dve", lambda e, h=h, sl=sl: e.tensor_tensor(kg[:, sl], kT[h][0][:, bl], Ebc[:, sl], ALU.mult), r=[kT[h][1], Ebck], w=[kgk])
                E("dve", lambda e, h=h, sl=sl: e.tensor_tensor(qd[:, sl], qT[h][0][:, bl], Ebc[:, sl], ALU.mult), r=[qT[h][1], Ebck], w=[qdk])
            pb, pbk = self.psumb()
            for h in range(4):
                E("pe", lambda e, h=h: e.transpose(pb[:, h * P:(h + 1) * P], vT[h][0][:, bl], self.ident_bf[:, 0, :]), r=[vT[h][1], "ident_bf"], w=[pbk])
            E("act", lambda e: e.activation(vtok, pb[:, 0:512], AF.Copy), r=[pbk], w=[vtokk])
            pb2, pb2k = self.psumb()
            for h in range(4):
                E("pe", lambda e, h=h: e.transpose(pb2[:, h * P:(h + 1) * P], kT[h][0][:, bl], self.ident_bf[:, 0, :]), r=[kT[h][1], "ident_bf"], w=[pb2k])
            for h in range(4):
                sl = slice(h * P, (h + 1) * P)
                E("dve", lambda e, h=h, sl=sl: e.tensor_scalar(kdec[:, sl], pb2[:, sl], Dm[:, h * P + P - 1:h * P + P], None, ALU.mult),
                  r=[pb2k, Dmk], w=[kdeck])
            pkS, pkSk = self.psum()
            for h in range(4):
                sl = slice(h * P, (h + 1) * P)
                E("pe", lambda e, h=h, sl=sl: e.matmul(pkS[:, sl], kg[:, sl], L["Sbf"][:, h, :], start=True, stop=True), r=[kgk, "gSbf" + sfx], w=[pkSk])
            E("dve", lambda e: e.tensor_tensor(Rb, vtok, pkS[:, :], ALU.subtract), r=[vtokk, pkSk], w=[Rbk])
            pvn, pvnk = self.psum()
            for h in range(4):
                sl = slice(h * P, (h + 1) * P)
                E("pe", lambda e, h=h, sl=sl: e.matmul(pvn[:, sl], W[:, sl], Rb[:, sl], start=True, stop=True), r=[Wk, Rbk], w=[pvnk])
            for h in range(4):
                sl = slice(h * P, (h + 1) * P)
                E("dve", lambda e, h=h, sl=sl: e.tensor_scalar(vn[:, sl], pvn[:, sl], sm[:, 0, j * 4 + h:j * 4 + h + 1], None, ALU.mult),
                  r=[pvnk, "od_sm0"], w=[vnk])
            po, pok = self.psum()
            for h in range(4):
                sl = slice(h * P, (h + 1) * P)
                E("pe", lambda e, h=h, sl=sl: e.matmul(po[:, sl], qd[:, sl], L["Sbf"][:, h, :], start=True, stop=False), r=[qdk, "gSbf" + sfx], w=[pok])
                E("pe", lambda e, h=h, sl=sl: e.matmul(po[:, sl], MT[:, sl], vn[:, sl], start=False, stop=True), r=[MTk, vnk], w=[pok])
            if gstop <= 5:
                return
            of, ofk = self.fs(3)
            E("act", lambda e: e.activation(of, po[:, :], AF.Copy), r=[pok], w=[ofk])
            pS, pSk = self.psum()
            for h in range(4):
                sl = slice(h * P, (h + 1) * P)
                E("pe", lambda e, h=h, sl=sl: e.matmul(pS[:, sl], kdec[:, sl], vn[:, sl], start=True, stop=True), r=[kdeck, vnk], w=[pSk])
            for h in range(4):
                sl = slice(h * P, (h + 1) * P)
                E("dve", lambda e, h=h, sl=sl: e.scalar_tensor_tensor(L["S"][:, h, :], L["S"][:, h, :], Ebc[:, h * P + P - 1:h * P + P], pS[:, sl], ALU.mult, ALU.add),
                  r=["gS" + sfx, Ebck, pSk], w=["gS" + sfx])
            E("pool", lambda e: e.tensor_copy(L["Sbf"][:, :, :], L["S"][:, :, :]), r=["gS" + sfx], w=["gSbf" + sfx])
            self.head_rmsnorm_out(of, ofk, L["ngg"], "ngg" + sfx, zs[j][0], zs[j][1], 0, bl)

    def mlstm(self, l):
        E = self.E
        L = self.L[l]
        sfx = "%d" % l
        sm = self.od_sm
        vaug = self.od_vaug
        qk = [self.bs(c) for c in range(4)]
        ogs = [self.bs(4 + b) for b in range(4)]
        w, wk = self.get_w()
        for c in range(4):
            pst, pk = self.proj_fm(w, wk, c * P)
            self.conv_silu(l, pst, pk, L["m_cc"], "m_cc" + sfx, c, "mconvw" + sfx, 4, qk[c][0], qk[c][1],
                           scale=(64.0 ** -0.5) if c < 2 else 1.0)
        w, wk = self.get_w()
        for b in range(4):
            pst, pk = self.proj_tm(w, wk, 0, 512, b)
            E("act", lambda e, b=b, pst=pst: e.activation(vaug[:, b, :, 0:P], pst[:, :].rearrange("p (h v) -> p h v", v=P), AF.Copy),
              r=[pk], w=["od_vaug"])
        w, wk = self.get_w()
        for b in range(4):
            pst, pk = self.proj_tm(w, wk, 0, 512, b)
            E("act", lambda e, b=b, pst=pst: e.activation(ogs[b][0], pst[:, :], AF.Sigmoid), r=[pk], w=[ogs[b][1]])
        mstop = getattr(self, "mstop", 99)
        if mstop < 99:
            for c in range(4, 8):
                self.E("pool", lambda e, c=c: e.memset(self.ymT[:, c, :], 0.0), w=["ymT.%d" % c])
        if mstop <= 1:
            return
        WmT, WmTk = self.bs(8)
        wkt, wktk = self.od_wk, "od_wk"
        qdt, qdtk = self.od_qd, "od_qd"
        for j in range(4):
            bl = slice(j * P, (j + 1) * P)
            pbc, pbck = self.bcast_cum(7, j)
            Ebc, Ebck = self.fs(0)
            E("act", lambda e: e.activation(Ebc, pbc[:, :], AF.Exp), r=[pbck], w=[Ebck])
            Dm, Dmk = self.fs(1)
            for h in range(4):
                E("act", lambda e, h=h: e.activation(Dm[:, h * P:(h + 1) * P], pbc[:, h * P:(h + 1) * P], AF.Exp,
                                                     bias=sm[:, 9, j * 4 + h:j * 4 + h + 1]),
                  r=[pbck, "od_sm9"], w=[Dmk])
            E("dve", lambda e: e.tensor_scalar(Dm, Dm, 1.0, None, ALU.min), r=[Dmk], w=[Dmk])
            for h in range(4):
                sl = slice(h * P, (h + 1) * P)
                E("dve", lambda e, h=h, sl=sl: e.scalar_tensor_tensor(Dm[:, sl], Dm[:, sl], sm[:, 10, j * 4 + h:j * 4 + h + 1],
                                                                      self.maskI[:, 0, :], ALU.mult, ALU.mult),
                  r=[Dmk, "od_sm10", "maskI"], w=[Dmk])
            if mstop <= 2:
                return
            pkq, pkqk = self.psum()
            for h in range(4):
                c, hh = h // 2, h % 2
                ps_ = slice(hh * 64, (hh + 1) * 64)
                sl = slice(h * P, (h + 1) * P)
                E("dve", lambda e: e.tensor_copy(self.od_qm[ps_, h, :], qk[c][0][ps_, bl]), r=[qk[c][1]], w=["od_qm"])
                E("pe", lambda e: e.matmul(pkq[:, sl], qk[2 + c][0][:, bl], self.od_qm[:, h, :], start=True, stop=True),
                  r=[qk[2 + c][1], "od_qm"], w=[pkqk])
            E("dve", lambda e: e.tensor_tensor(WmT, pkq[:, :], Dm, ALU.mult), r=[pkqk, Dmk], w=[WmTk])
            if mstop <= 2.3:
                return
            for h in range(4):
                c, hh = h // 2, h % 2
                ps_ = slice(hh * 64, (hh + 1) * 64)
                E("dve", lambda e: e.tensor_tensor(qdt[ps_, h, :], qk[c][0][ps_, bl], Ebc[ps_, h * P:(h + 1) * P], ALU.mult),
                  r=[qk[c][1], Ebck], w=[qdtk])
            if mstop <= 2.6:
                return
            pb, pbk = self.psumb()
            for c in range(2):
                E("pe", lambda e, c=c: e.transpose(pb[:, c * P:(c + 1) * P], qk[2 + c][0][:, bl], self.ident_bf[:, 0, :]), r=[qk[2 + c][1], "ident_bf"], w=[pbk])
            for h in range(4):
                E("dve", lambda e, h=h: e.tensor_scalar(wkt[:, h, :], pb[:, h * 64:(h + 1) * 64], Dm[:, h * P + P - 1:h * P + P], None, ALU.mult),
                  r=[pbk, Dmk], w=[wktk])
            if mstop <= 3:
                return
            ho, hok = self.fs(3)
            st, stk = self.rstd, "rstd"
            for pr in range(2):
                pn, pnk = self.psum()
                for hh in range(2):
                    h = pr * 2 + hh
                    ps_ = slice(hh * 64, (hh + 1) * 64)
                    cs = slice(hh * 129, hh * 129 + 129)
                    E("pe", lambda e: e.matmul(pn[:, cs], qdt[:, h, :], L["Cbf"][:, pr, 0:129], start=True, stop=False),
                      r=[qdtk, "mCbf" + sfx], w=[pnk])
                    E("pe", lambda e: e.matmul(pn[:, cs], WmT[:, h * P:(h + 1) * P], vaug[:, j, h, 0:129], start=False, stop=True),
                      r=[WmTk, "od_vaug"], w=[pnk])
                for hh in range(2):
                    h = pr * 2 + hh
                    E("act", lambda e: e.activation(st[:, 8 + h:9 + h], pn[:, hh * 129 + 128:hh * 129 + 129], AF.Abs), r=[pnk], w=[stk])
                    E("dve", lambda e: e.tensor_scalar(st[:, 8 + h:9 + h], st[:, 8 + h:9 + h], 1.0, None, ALU.max), r=[stk], w=[stk])
                    E("dve", lambda e: e.reciprocal(st[:, 8 + h:9 + h], st[:, 8 + h:9 + h]), r=[stk], w=[stk])
                    E("dve", lambda e: e.tensor_scalar(ho[:, h * P:(h + 1) * P], pn[:, hh * 129:hh * 129 + 128], st[:, 8 + h:9 + h], None, ALU.mult),
                      r=[pnk, stk], w=[hok])
            if mstop <= 4:
                return
            for h in range(4):
                c, hh = h // 2, h % 2
                ps_ = slice(hh * 64, (hh + 1) * 64)
                pC, pCk = self.psum()
                E("pe", lambda e: e.matmul(pC[:, 0:129], wkt[:, 2 * c:2 * c + 2, :].rearrange("p a d -> p (a d)"), vaug[:, j, h, 0:129], start=True, stop=True),
                  r=[wktk, "od_vaug"], w=[pCk])
                E("dve", lambda e: e.scalar_tensor_tensor(L["C"][ps_, c, 0:129], L["C"][ps_, c, 0:129], Ebc[ps_, h * P + P - 1:h * P + P], pC[ps_, 0:129], ALU.mult, ALU.add),
                  r=["mC" + sfx, Ebck, pCk], w=["mC" + sfx])
            E("pool", lambda e: e.tensor_copy(L["Cbf"][:, :, :], L["C"][:, :, :]), r=["mC" + sfx], w=["mCbf" + sfx])
            if mstop <= 5:
                return
            self.head_rmsnorm_out(ho, hok, L["ngm"], "ngm" + sfx, ogs[j][0], ogs[j][1], 4, bl)


def run_cfg(T, layers, inputs_per_core, n_cores, trace=False, skip=()):
    mk = MK(T, layers)
    mk.skip = skip
    import os
    mk.gstop = float(os.environ.get('GSTOP', '99'))
    mk.mstop = float(os.environ.get('MSTOP', '99'))
    nc = mk.build()
    maps = [{k: m[k] for k in mk.in_names} for m in inputs_per_core]
    res = run_bass_kernel_spmd(nc, maps, core_ids=list(range(n_cores)), trace=trace)
    return res


EV_NAMES = ["lru_conv_w", "lru_conv_b", "lru_wa", "lru_ba", "lru_wx", "lru_bx", "lru_lambda", "rwkv_mu", "rwkv_w0",
            "rwkv_w2", "rwkv_a0", "rwkv_a2", "rwkv_g2", "rwkv_kk", "rwkv_ka", "rwkv_rk", "rwkv_lnw", "rwkv_lnb"]
OD_NAMES = ["gdn_conv_w", "gdn_a_log", "gdn_dt_bias", "gdn_norm_g", "mlstm_conv_w", "mlstm_ig_b", "mlstm_fg_b",
            "mlstm_norm_g"]


def const_masks():
    import ml_dtypes
    j = np.arange(P)[:, None]
    i = np.arange(P)[None, :]
    ms = [(j // 4 == i // 4)]
    offs = []
    for b in (4, 8, 16, 32, 64):
        offs.append((j // (2 * b) == i // (2 * b)) & (j % (2 * b) < b) & (i % (2 * b) >= b))
    ms += offs + [o.T for o in offs]
    return np.ascontiguousarray(np.concatenate([m_.astype(np.float32) for m_ in ms], axis=1).astype(ml_dtypes.bfloat16))


def core_inputs(inp, x2d, layers):
    c = lambda a: np.ascontiguousarray(np.asarray(a, dtype=np.float32))
    m = {"x": c(x2d), "final_g": c(inp["final_g"]), "cmask": const_masks()}
    for (kind, l) in layers:
        s = "%d" % l
        m["mlp_up" + s] = c(inp["mlp_up"][l])
        m["mlp_down" + s] = c(inp["mlp_down"][l])
        m["norm_mix_g" + s] = c(inp["norm_mix_g"][l])
        m["norm_mlp_g" + s] = c(inp["norm_mlp_g"][l])
        if kind == "even":
            e = l // 2
            m["ev_w_in" + s] = c(inp["ev_w_in"][e])
            m["ev_w_out" + s] = c(inp["ev_w_out"][e])
            for n in EV_NAMES:
                m[n + s] = c(np.asarray(inp[n][e]).reshape(-1) if n in ("lru_conv_w", "rwkv_rk") else inp[n][e])
        elif kind == "odd":
            o = l // 2
            m["od_w_in" + s] = c(inp["od_w_in"][o])
            m["od_w_out" + s] = c(inp["od_w_out"][o])
            for n in OD_NAMES:
                m[n + s] = c(np.asarray(inp[n][o]).reshape(-1))
    return m


LAYERS = [("even", 0), ("odd", 1), ("even", 2), ("odd", 3)]


def kernel(**inputs):
    x = np.asarray(inputs["x"], dtype=np.float32)
    B, T, _ = x.shape
    maps = [core_inputs(inputs, x[b], LAYERS) for b in range(B)]
    res = run_cfg(T, LAYERS, maps, B)
    return np.stack([np.asarray(r["out"], dtype=np.float32) for r in res.results], axis=0)
```
